# Optimizing a Trainium2 kernel written in Bass

```python
import jax, jax.numpy as jnp
from jax import lax
import numpy as np

D_MODEL = 1024
BATCH = 32
SEQ = 2048
DEPTH = 4

CHUNK = 64
D_RWKV = D_MODEL // 2
RWKV_HEAD = 64
RWKV_HEADS = D_RWKV // RWKV_HEAD
LORA_W = 64
LORA_A = 64
LORA_V = 32
LORA_G = 160
RWKV_GN_EPS = 64e-5
D_MLSTM = D_MODEL // 2
MLSTM_HEADS = 4
MLSTM_HEAD = D_MLSTM // MLSTM_HEADS
CONV_W = 4
MLSTM_NORM_EPS = 1e-5
D_FF = ((8 * D_MODEL // 3 + 255) // 256) * 256
NORM_EPS = 1e-6
HALF = 0.5

RW_SPLITS = (D_RWKV, D_RWKV, D_RWKV, LORA_W, LORA_A, LORA_G)
ML_SPLITS = (D_MLSTM, D_MLSTM, D_MLSTM, D_MLSTM, MLSTM_HEADS, MLSTM_HEADS)
N_RW_IN = 3 * D_RWKV + LORA_W + LORA_A + LORA_G
N_ML_IN = 4 * D_MLSTM + 2 * MLSTM_HEADS
N_GATE = 2 * D_MODEL
N_IN = N_RW_IN + N_ML_IN + N_GATE

kernel_name = "hybrid_rwkv7_mlstm_macaron"


def _rmsnorm(x, g):
    xf = x.astype(jnp.float32)
    y = xf * lax.rsqrt(jnp.mean(xf * xf, axis=-1, keepdims=True) + NORM_EPS)
    return (y * g.astype(jnp.float32)).astype(x.dtype)


def _swiglu(x, w_in, w_out):
    gate, up = jnp.split(x @ w_in, 2, axis=-1)
    return (jax.nn.silu(gate) * up) @ w_out


def _time_shift(z):
    return jnp.pad(z, ((0, 0), (1, 0), (0, 0)))[:, :-1]


def _causal_dwconv(z, w, b):
    K, T = w.shape[0], z.shape[1]
    zp = jnp.pad(z, ((0, 0), (K - 1, 0), (0, 0)))
    out = b
    for j in range(K):
        out = out + w[j] * zp[:, j:j + T]
    return out


def _split_cols(z, sizes):
    out, start = [], 0
    for s in sizes:
        out.append(z[..., start:start + s])
        start += s
    return out


def _head_standardize(y, eps):
    yf = y.astype(jnp.float32)
    mu = jnp.mean(yf, axis=-1, keepdims=True)
    var = jnp.mean(jnp.square(yf - mu), axis=-1, keepdims=True)
    yn = (yf - mu) * lax.rsqrt(var + eps)
    return yn.reshape(y.shape[0], y.shape[1], -1)


def _rwkv7_recurrence(r, w, k, v, kk, b):
    Bn, T, H, N = r.shape
    xs = tuple(jnp.moveaxis(t.astype(jnp.float32), 1, 0) for t in (r, w, k, v, kk, b))

    def step(S, inp):
        r_t, w_t, k_t, v_t, kk_t, b_t = inp
        sa = jnp.einsum('bhvk,bhk->bhv', S, kk_t)
        S = (S * w_t[:, :, None, :]
             - sa[..., :, None] * b_t[:, :, None, :]
             + v_t[..., :, None] * k_t[:, :, None, :])
        return S, jnp.einsum('bhvk,bhk->bhv', S, r_t)

    S0 = jnp.zeros((Bn, H, N, N), jnp.float32)
    _, ys = lax.scan(step, S0, xs)
    return jnp.moveaxis(ys, 0, 1)


def _rwkv7_mixer(r, k, v, xw, xa, xg, w0, w_up, a0, a_up, g_up, k_k, k_a, r_k, gn_w, gn_b):
    Bn, T, _ = r.shape
    heads = lambda t: t.reshape(Bn, T, RWKV_HEADS, RWKV_HEAD)
    w_log = -jax.nn.softplus(-(w0 + jnp.tanh(xw) @ w_up)) - 0.5
    decay = jnp.exp(-jnp.exp(w_log.astype(jnp.float32)))
    a = jax.nn.sigmoid(a0 + xa @ a_up)
    g = jax.nn.sigmoid(xg) @ g_up
    kk = heads(k * k_k).astype(jnp.float32)
    kk = kk / jnp.maximum(jnp.sqrt(jnp.sum(kk * kk, axis=-1, keepdims=True)), 1e-12)
    k = k * (1.0 + (a - 1.0) * k_a)
    rh, kh, vh, ah = heads(r), heads(k), heads(v), heads(a)
    y = _rwkv7_recurrence(rh, heads(decay), kh, vh, kk, kk * ah.astype(jnp.float32))
    y = _head_standardize(y, RWKV_GN_EPS) * gn_w + gn_b
    bonus = (jnp.sum(rh * kh * r_k, axis=-1, keepdims=True) * vh).reshape(Bn, T, -1)
    return ((y + bonus) * g).astype(r.dtype)


def _mlstm_chunkwise(q, k, v, i_pre, f_pre):
    Bn, T, H, d = q.shape
    nc = T // CHUNK
    to_chunks = lambda t: t.astype(jnp.float32).reshape(Bn, nc, CHUNK, H, -1).transpose(1, 0, 3, 2, 4)
    qc = to_chunks(q) * (d ** -0.5)
    kc, vc = to_chunks(k), to_chunks(v)
    lic = to_chunks(i_pre[..., None])[..., 0]
    lfc = jax.nn.log_sigmoid(to_chunks(f_pre[..., None])[..., 0])
    causal = jnp.tril(jnp.ones((CHUNK, CHUNK), bool))

    def step(carry, inp):
        C, n, m = carry
        q_, k_, v_, li, lf = inp
        bcum = jnp.cumsum(lf, axis=-1)
        Dm = jnp.where(causal, bcum[..., :, None] - bcum[..., None, :] + li[..., None, :], -jnp.inf)
        inter = bcum + m[..., None]
        m_t = jnp.maximum(inter, jnp.max(Dm, axis=-1))
        inter_w = jnp.exp(inter - m_t)
        s = jnp.einsum('bhtd,bhsd->bhts', q_, k_) * jnp.exp(Dm - m_t[..., None])
        num = inter_w[..., None] * jnp.einsum('bhvk,bhtk->bhtv', C, q_) + jnp.einsum('bhts,bhsv->bhtv', s, v_)
        den = inter_w * jnp.einsum('bhk,bhtk->bht', n, q_) + jnp.sum(s, axis=-1)
        h = num / jnp.maximum(jnp.abs(den), jnp.exp(-m_t))[..., None]
        bL = bcum[..., -1]
        ws_log = bL[..., None] - bcum + li
        m_new = jnp.maximum(bL + m, jnp.max(ws_log, axis=-1))
        sdec = jnp.exp(bL + m - m_new)
        ws = jnp.exp(ws_log - m_new[..., None])
        C_new = sdec[..., None, None] * C + jnp.einsum('bhs,bhsv,bhsk->bhvk', ws, v_, k_)
        n_new = sdec[..., None] * n + jnp.einsum('bhs,bhsk->bhk', ws, k_)
        return (C_new, n_new, m_new), h

    carry0 = (jnp.zeros((Bn, H, d, d), jnp.float32), jnp.zeros((Bn, H, d), jnp.float32),
              jnp.zeros((Bn, H), jnp.float32))
    _, hs = lax.scan(step, carry0, (qc, kc, vc, lic, lfc))
    return hs.transpose(1, 0, 3, 2, 4).reshape(Bn, T, H, d)


def _mlstm_mixer(mq, mk, mv, mo, mi, mf, conv_w, conv_b, i_bias, f_bias, norm_w):
    Bn, T, _ = mq.shape
    heads = lambda t: t.reshape(Bn, T, MLSTM_HEADS, MLSTM_HEAD)
    qk = jax.nn.silu(_causal_dwconv(jnp.concatenate([mq, mk], axis=-1), conv_w, conv_b))
    q, k = jnp.split(qk, 2, axis=-1)
    h = _mlstm_chunkwise(heads(q), heads(k), heads(mv), mi + i_bias, mf + f_bias)
    h = _head_standardize(h, MLSTM_NORM_EPS) * norm_w
    return (jax.nn.sigmoid(mo) * h).astype(mq.dtype)


def setup_inputs(seed: int = 0) -> dict:
    key = jax.random.key(seed)
    ks = iter(jax.random.split(key, 48))
    f32 = jnp.float32
    L = DEPTH

    def nrm(shape, scale):
        return jax.random.normal(next(ks), shape, f32) * scale

    def gain(shape):
        return 1.0 + nrm(shape, 0.05)

    return {
        "x": nrm((BATCH, SEQ, D_MODEL), 1.0),
        "ffn1_norm": gain((L, D_MODEL)),
        "ffn1_w_in": nrm((L, D_MODEL, 2 * D_FF), D_MODEL ** -0.5),
        "ffn1_w_out": nrm((L, D_FF, D_MODEL), D_FF ** -0.5),
        "mix_norm": gain((L, D_MODEL)),
        "w_in": nrm((L, D_MODEL, N_IN), D_MODEL ** -0.5),
        "shift_mu": jax.random.uniform(next(ks), (L, N_RW_IN), f32, 0.1, 0.9),
        "rw_w0": jnp.linspace(-6.0, -1.0, D_RWKV, dtype=f32)[None, :] + nrm((L, D_RWKV), 0.1),
        "rw_w_up": nrm((L, LORA_W, D_RWKV), 0.5 * LORA_W ** -0.5),
        "rw_a0": nrm((L, D_RWKV), 0.1),
        "rw_a_up": nrm((L, LORA_A, D_RWKV), 0.5 * LORA_A ** -0.5),
        "rw_g_up": nrm((L, LORA_G, D_RWKV), LORA_G ** -0.5),
        "rw_k_k": 0.85 + nrm((L, D_RWKV), 0.05),
        "rw_k_a": gain((L, D_RWKV)),
        "rw_r_k": nrm((L, RWKV_HEADS, RWKV_HEAD), 0.1),
        "rw_gn_w": gain((L, D_RWKV)),
        "rw_gn_b": nrm((L, D_RWKV), 0.02),
        "vres_down": nrm((L - 1, D_MODEL, LORA_V), D_MODEL ** -0.5),
        "vres_up": nrm((L - 1, LORA_V, D_RWKV), 0.5 * LORA_V ** -0.5),
        "vres_bias": 1.0 + nrm((L - 1, D_RWKV), 0.1),
        "ml_conv_w": nrm((L, CONV_W, 2 * D_MLSTM), CONV_W ** -0.5),
        "ml_conv_b": nrm((L, 2 * D_MLSTM), 0.02),
        "ml_i_bias": nrm((L, MLSTM_HEADS), 0.1),
        "ml_f_bias": jnp.linspace(3.0, 6.0, MLSTM_HEADS, dtype=f32)[None, :] + nrm((L, MLSTM_HEADS), 0.1),
        "ml_norm_w": gain((L, D_MLSTM)),
        "br_a": nrm((L, D_RWKV, D_MODEL), D_RWKV ** -0.5),
        "br_b": nrm((L, D_MLSTM, D_MODEL), D_MLSTM ** -0.5),
        "w_out": nrm((L, D_MODEL, D_MODEL), D_MODEL ** -0.5),
        "ffn2_norm": gain((L, D_MODEL)),
        "ffn2_w_in": nrm((L, D_MODEL, 2 * D_FF), D_MODEL ** -0.5),
        "ffn2_w_out": nrm((L, D_FF, D_MODEL), D_FF ** -0.5),
        "final_norm": gain((D_MODEL,)),
    }


def reference(x, ffn1_norm, ffn1_w_in, ffn1_w_out, mix_norm, w_in, shift_mu,
              rw_w0, rw_w_up, rw_a0, rw_a_up, rw_g_up, rw_k_k, rw_k_a, rw_r_k, rw_gn_w, rw_gn_b,
              vres_down, vres_up, vres_bias,
              ml_conv_w, ml_conv_b, ml_i_bias, ml_f_bias, ml_norm_w,
              br_a, br_b, w_out, ffn2_norm, ffn2_w_in, ffn2_w_out, final_norm):
    h = x
    v_first = None
    for l in range(DEPTH):
        h = h + HALF * _swiglu(_rmsnorm(h, ffn1_norm[l]), ffn1_w_in[l], ffn1_w_out[l])

        xn = _rmsnorm(h, mix_norm[l])
        z = xn @ w_in[l]
        z_rw = z[..., :N_RW_IN]
        z_ml = z[..., N_RW_IN:N_RW_IN + N_ML_IN]
        z_gate = z[..., N_RW_IN + N_ML_IN:]

        z_rw = z_rw + shift_mu[l] * (_time_shift(z_rw) - z_rw)
        r, k, v, xw, xa, xg = _split_cols(z_rw, RW_SPLITS)
        if l == 0:
            v_first = v
        else:
            vg = jax.nn.sigmoid(vres_bias[l - 1] + (xn @ vres_down[l - 1]) @ vres_up[l - 1])
            v = v + (v_first - v) * vg
        y_a = _rwkv7_mixer(r, k, v, xw, xa, xg, rw_w0[l], rw_w_up[l], rw_a0[l], rw_a_up[l],
                           rw_g_up[l], rw_k_k[l], rw_k_a[l], rw_r_k[l], rw_gn_w[l], rw_gn_b[l])

        mq, mk, mv, mo, mi, mf = _split_cols(z_ml, ML_SPLITS)
        y_b = _mlstm_mixer(mq, mk, mv, mo, mi, mf, ml_conv_w[l], ml_conv_b[l],
                           ml_i_bias[l], ml_f_bias[l], ml_norm_w[l])

        g_a, g_b = jnp.split(z_gate, 2, axis=-1)
        u = jax.nn.sigmoid(g_a) * (y_a @ br_a[l]) + jax.nn.sigmoid(g_b) * (y_b @ br_b[l])
        h = h + u @ w_out[l]

        h = h + HALF * _swiglu(_rmsnorm(h, ffn2_norm[l]), ffn2_w_in[l], ffn2_w_out[l])
    return _rmsnorm(h, final_norm)
```

```python
import numpy as np
from contextlib import ExitStack
import concourse.bass as bass
import concourse.mybir as mybir
from concourse.bass_utils import run_bass_kernel_spmd

F32 = mybir.dt.float32
BF16 = mybir.dt.bfloat16
AF = mybir.ActivationFunctionType
ALU = mybir.AluOpType
MULT, ADD, SUB, MAXOP = ALU.mult, ALU.add, ALU.subtract, ALU.max
EPOCH = 16000
ENGS = ("pe", "act", "dve", "pool", "sp")

D = 1024
DFF = 2816
NIN = 5928
C0 = float(np.exp(-0.5))


class Reg:
    __slots__ = ("ap", "w", "rs", "name", "track", "psum")

    def __init__(self, ap, name="", track=True, psum=False):
        self.ap = ap
        self.w = []
        self.rs = {}
        self.name = name
        self.track = track
        self.psum = psum

    def __getitem__(self, idx):
        return View(self, self.ap[idx])

    @property
    def reg(self):
        return self

    def v(self, ap):
        return View(self, ap)


class View:
    __slots__ = ("reg", "ap")

    def __init__(self, reg, ap):
        self.reg = reg
        self.ap = ap

    def __getitem__(self, idx):
        return View(self.reg, self.ap[idx])

    def rearrange(self, *a, **k):
        return View(self.reg, self.ap.rearrange(*a, **k))

    def bitcast(self, dt):
        return View(self.reg, self.ap.bitcast(dt))


def tok_key(tok):
    return tok[0] if tok[0] in ENGS else ("d", tok[1])


class Sched:
    def __init__(self, nc, n_dma_sems=32, same_engine_sync=True):
        self.nc = nc
        self.ops = {e: [] for e in ENGS}
        self.count = {e: 0 for e in ENGS}
        self.waited = {e: {} for e in ENGS}
        self.n_dma_sems = n_dma_sems
        self.dma_cnt = [0] * n_dma_sems
        self.dma_rr = 0
        self.same_engine_sync = same_engine_sync
        self.needed = {e: set() for e in ENGS}
        self.final_waits = []

    def _need(self, eng, tok, waits):
        if tok is None:
            return
        key = tok_key(tok)
        val = tok[-1]
        if key == eng and (eng == "pe" or not self.same_engine_sync):
            return
        if self.waited[eng].get(key, -1) >= val:
            return
        if key in waits and waits[key][-1] >= val:
            return
        waits[key] = tok

    def _deps(self, eng, reads, writes, waits=None):
        waits = {} if waits is None else waits
        for r in reads:
            if r.reg.track:
                for t in r.reg.w:
                    self._need(eng, t, waits)
                if r.reg.psum:
                    for k_, t in r.reg.rs.items():
                        if k_ != eng:
                            self._need(eng, t, waits)
        for w in writes:
            if w.reg.track:
                for t in w.reg.w:
                    self._need(eng, t, waits)
                for t in w.reg.rs.values():
                    self._need(eng, t, waits)
        for key, tok in waits.items():
            self.waited[eng][key] = tok[-1]
            if tok[0] in ENGS:
                self.needed[tok[0]].add(tok[1])
        return list(waits.values())

    def _commit(self, tok, reads, writes, append_w=False):
        for r in reads:
            if r.reg.track:
                r.reg.rs[tok_key(tok)] = tok
        for w in writes:
            if w.reg.track:
                if append_w:
                    w.reg.w = w.reg.w + [tok]
                else:
                    w.reg.w = [tok]
                w.reg.rs = {}

    def op(self, eng, fn, reads=(), writes=()):
        waits = self._deps(eng, reads, writes)
        idx = self.count[eng]
        self.count[eng] += 1
        tok = (eng, idx)
        self.ops[eng].append((waits, fn, tok, "c"))
        self._commit(tok, reads, writes)
        return tok

    def dma(self, eng, out, in_, join=False):
        k = self.dma_rr
        self.dma_rr = (self.dma_rr + 1) % self.n_dma_sems
        waits = {}
        if self.dma_cnt[k]:
            self._need(eng, ("dma", k, 16 * self.dma_cnt[k]), waits)
        reads, writes = [in_], [out]
        if join:
            saved = out.reg.w
            out.reg.w = []
            allw = self._deps(eng, reads, writes, waits)
            out.reg.w = saved
        else:
            allw = self._deps(eng, reads, writes, waits)
        self.dma_cnt[k] += 1
        tok = ("dma", k, 16 * self.dma_cnt[k])
        oap, iap = out.ap, in_.ap
        self.count[eng] += 1
        self.ops[eng].append((allw, lambda e: e.dma_start(out=oap, in_=iap), tok, "d"))
        self._commit(tok, reads, writes, append_w=join)
        return tok

    def transfer_hazards(self, src_regs, dst_regs):
        merged = {}
        for r in src_regs:
            for t in list(r.rs.values()) + list(r.w):
                key = tok_key(t)
                if key not in merged or merged[key][-1] < t[-1]:
                    merged[key] = t
        for d in dst_regs:
            for key, t in merged.items():
                if key not in d.rs or d.rs[key][-1] < t[-1]:
                    d.rs[key] = t

    def wait_all_at_end(self, eng, toks):
        self.final_waits.append((eng, list(toks)))

    def emit(self, st):
        nc = self.nc
        rank, sems = {}, {}
        for e in ENGS:
            ms = sorted(self.needed[e])
            rank[e] = {i: r + 1 for r, i in enumerate(ms)}
            n_ep = max(1, (len(ms) + EPOCH - 1) // EPOCH)
            sems[e] = [st.enter_context(nc.semaphore(f"s_{e}_{k}")) for k in range(n_ep)]
        dsems = [st.enter_context(nc.semaphore(f"s_dma_{k}")) for k in range(self.n_dma_sems)]

        def resolve(tok):
            if tok[0] == "dma":
                return dsems[tok[1]], tok[2]
            e, i = tok
            r = rank[e][i]
            return sems[e][(r - 1) // EPOCH], (r - 1) % EPOCH + 1

        block = st.enter_context(nc.Block())

        def make_body(e):
            def body(eng):
                for waits, fn, tok, kind in self.ops[e]:
                    ws = [resolve(w) for w in waits]
                    for s, v in ws[1:]:
                        eng.wait_ge(s, v)
                    ins = fn(eng)
                    if ws:
                        ins = ins._wait_ge(ws[0][0], ws[0][1])
                    if kind == "d":
                        ins.then_inc(dsems[tok[1]], 16)
                    elif tok[1] in rank[e]:
                        s, v = resolve(tok)
                        ins.then_inc(s, 1)
                for (fe, toks) in self.final_waits:
                    if fe == e:
                        for t in toks:
                            s, v = resolve(t)
                            eng.wait_ge(s, v)
            return body

        block.tensor(make_body("pe"))
        block.scalar(make_body("act"))
        block.vector(make_body("dve"))
        block.gpsimd(make_body("pool"))
        block.sync(make_body("sp"))


class PsumPool:
    def __init__(self, regs):
        self.free = list(regs)

    def alloc(self):
        assert self.free, "PSUM pool exhausted"
        return self.free.pop(0)

    def release(self, r):
        self.free.append(r)


PV_FFN1, PV_MIX, PV_FFN2, PV_FIN = 0, 8, 16, 24
PV_MU = 32
PV_W0, PV_A0, PV_KK, PV_KA, PV_RK, PV_GNW, PV_GNB, PV_VB = 47, 51, 55, 59, 63, 67, 71, 75
PV_CW, PV_CB, PV_IB, PV_FB, PV_NW = 79, 111, 119, 120, 121
NPV = 128


def _cols(vec, n):
    out = np.zeros((128, n), np.float32)
    v = np.asarray(vec, np.float32).reshape(-1)
    full = len(v) // 128
    if full:
        out[:, :full] = v[:full * 128].reshape(full, 128).T
    rem = len(v) - full * 128
    if rem:
        out[:rem, full] = v[full * 128:]
    return out


def pack_params(inp, depth):
    pv = np.zeros((128, depth, NPV), np.float32)
    for l in range(depth):
        p = pv[:, l]
        p[:, PV_FFN1:PV_FFN1 + 8] = _cols(inp["ffn1_norm"][l], 8)
        p[:, PV_MIX:PV_MIX + 8] = _cols(inp["mix_norm"][l], 8)
        p[:, PV_FFN2:PV_FFN2 + 8] = _cols(inp["ffn2_norm"][l], 8)
        p[:, PV_FIN:PV_FIN + 8] = _cols(inp["final_norm"], 8)
        mu = inp["shift_mu"][l]
        p[:, PV_MU:PV_MU + 12] = _cols(mu[0:1536], 12)
        p[:, PV_MU + 12:PV_MU + 13] = _cols(mu[1536:1664], 1)
        p[:, PV_MU + 13:PV_MU + 14] = _cols(mu[1664:1792], 1)
        p[:, PV_MU + 14:PV_MU + 15] = _cols(mu[1792:1824], 1)
        p[:, PV_W0:PV_W0 + 4] = _cols(inp["rw_w0"][l], 4)
        p[:, PV_A0:PV_A0 + 4] = _cols(inp["rw_a0"][l], 4)
        p[:, PV_KK:PV_KK + 4] = _cols(inp["rw_k_k"][l], 4)
        p[:, PV_KA:PV_KA + 4] = _cols(inp["rw_k_a"][l], 4)
        p[:, PV_RK:PV_RK + 4] = _cols(inp["rw_r_k"][l].reshape(-1), 4)
        p[:, PV_GNW:PV_GNW + 4] = _cols(inp["rw_gn_w"][l], 4)
        p[:, PV_GNB:PV_GNB + 4] = _cols(inp["rw_gn_b"][l], 4)
        if l > 0:
            p[:, PV_VB:PV_VB + 4] = _cols(inp["vres_bias"][l - 1], 4)
        for j in range(4):
            p[:, PV_CW + 8 * j:PV_CW + 8 * j + 8] = _cols(inp["ml_conv_w"][l][j], 8)
        p[:, PV_CB:PV_CB + 8] = _cols(inp["ml_conv_b"][l], 8)
        p[0:4, PV_IB] = inp["ml_i_bias"][l]
        p[0:4, PV_FB] = inp["ml_f_bias"][l]
        p[:, PV_NW:PV_NW + 4] = _cols(inp["ml_norm_w"][l], 4)
    return pv


def make_consts():
    c = np.zeros((128, 7, 512), np.float32)
    c[:, 0, 0:128] = np.eye(128)
    bd = np.zeros((128, 128), np.float32)
    bd[0:64, 0:64] = 1.0
    bd[64:128, 64:128] = 1.0
    c[:, 0, 128:256] = bd
    s = np.arange(64)[:, None]
    t = np.arange(64)[None, :]
    strict = (s < t).astype(np.float32)
    incl = (s <= t).astype(np.float32)
    lower = (s > t).astype(np.float32)
    c[0:64, 1] = np.tile(np.concatenate([strict, incl], axis=1), (1, 4))
    c[0:64, 2] = np.tile(lower, (1, 8))
    c[0:64, 3] = np.tile(incl, (1, 8))
    c[0:64, 4] = np.tile(np.eye(64, dtype=np.float32), (1, 8))
    r = np.ones((128, 512), np.float32)
    r[:, ::64] = 0.0
    c[:, 5] = r
    for h in range(4):
        c[h, 6, h * 128:(h + 1) * 128] = 1.0
    return c


class Cfg:
    def __init__(self, NS=4, T=2048, DEPTH=4, NTB=1, do_ffn=True, do_rw=True, do_ml=True, do_mix=True,
                 prepass=True):
        self.NS, self.T, self.DEPTH, self.NTB = NS, T, DEPTH, NTB
        self.prepass = prepass
        self.do_ffn, self.do_rw, self.do_ml, self.do_mix = do_ffn, do_rw, do_ml, do_mix


def build_program(cfg):
    NS, T, DEPTH, NTB = cfg.NS, cfg.T, cfg.DEPTH, cfg.NTB
    SEG = NTB * 512
    NSEG = T // SEG
    nc = bass.Bass("TRN2", target_bir_lowering=False)

    def din(name, shape):
        return Reg(nc.dram_tensor(name, list(shape), F32, kind="ExternalInput").ap(), name, track=False)

    xT = din("xT", [NS, D, T])
    d_ffn1_in = din("ffn1_w_in", [DEPTH, D, 2 * DFF])
    d_ffn1_out = din("ffn1_w_out", [DEPTH, DFF, D])
    d_ffn2_in = din("ffn2_w_in", [DEPTH, D, 2 * DFF])
    d_ffn2_out = din("ffn2_w_out", [DEPTH, DFF, D])
    d_win = din("w_in", [DEPTH, D, NIN])
    d_wup = din("rw_w_up", [DEPTH, 64, 512])
    d_aup = din("rw_a_up", [DEPTH, 64, 512])
    d_gup = din("rw_g_up", [DEPTH, 160, 512])
    d_vd = din("vres_down", [max(DEPTH - 1, 1), D, 32])
    d_vu = din("vres_up", [max(DEPTH - 1, 1), 32, 512])
    d_bra = din("br_a", [DEPTH, 512, D])
    d_brb = din("br_b", [DEPTH, 512, D])
    d_wout = din("w_out", [DEPTH, D, D])
    d_pv = din("pvec", [128, DEPTH, NPV])
    d_cst = din("consts", [128, 7, 512])
    outT = Reg(nc.dram_tensor("outT", [NS, D, T], F32, kind="ExternalOutput").ap(), "outT", track=False)

    S = Sched(nc)
    st = ExitStack()
    with st:
        cnt = [0]

        def sb(shape, dt=F32, name=None):
            cnt[0] += 1
            nm = name or f"t{cnt[0]}"
            return st.enter_context(nc.sbuf_tensor(nm, list(shape), dt))[:]

        def R(shape, dt=F32, name=None):
            return Reg(sb(shape, dt, name), name or "")

        def rd(x, lst):
            if hasattr(x, "reg"):
                lst.append(x)
                return x.ap
            return x

        def mm(out, lhsT, rhs, start=True, stop=True):
            o, l, r = out.ap, lhsT.ap, rhs.ap
            S.op("pe", lambda e: e.matmul(o, l, r, start=start, stop=stop), [lhsT, rhs], [out])

        def act(out, in_, func, bias=None, scale=None):
            reads = [in_]
            kw = {}
            if bias is not None:
                kw["bias"] = rd(bias, reads)
            if scale is not None:
                kw["scale"] = rd(scale, reads)
            o, i = out.ap, in_.ap
            S.op("act", lambda e: e.activation(o, i, func, **kw), reads, [out])

        def ts(eng, out, in0, s1, s2=None, op0=MULT, op1=None):
            reads = [in0]
            a1 = rd(s1, reads)
            a2 = rd(s2, reads) if s2 is not None else None
            o, i = out.ap, in0.ap
            if op1 is None:
                S.op(eng, lambda e: e.tensor_scalar(o, i, a1, None, op0), reads, [out])
            else:
                S.op(eng, lambda e: e.tensor_scalar(o, i, a1, a2, op0, op1), reads, [out])

        def tt(eng, out, in0, in1, op):
            o, a, b = out.ap, in0.ap, in1.ap
            S.op(eng, lambda e: e.tensor_tensor(o, a, b, op), [in0, in1], [out])

        def stt(eng, out, in0, scalar, in1, op0, op1):
            reads = [in0, in1]
            sc = rd(scalar, reads)
            o, a, b = out.ap, in0.ap, in1.ap
            S.op(eng, lambda e: e.scalar_tensor_tensor(o, a, sc, b, op0, op1), reads, [out])

        def cp(eng, out, in_):
            o, i = out.ap, in_.ap
            if eng == "act":
                S.op("act", lambda e: e.activation(o, i, AF.Copy), [in_], [out])
            else:
                S.op(eng, lambda e: e.tensor_copy(o, i), [in_], [out])

        def memset(eng, out, val):
            o = out.ap
            S.op(eng, lambda e: e.memset(o, val), [], [out])

        def recip(out, in_):
            o, i = out.ap, in_.ap
            S.op("dve", lambda e: e.reciprocal(o, i), [in_], [out])

        def scan(out, d0, d1):
            o, a, b = out.ap, d0.ap, d1.ap
            S.op("dve", lambda e: e.tensor_tensor_scan(o, a, b, 0.0, MULT, ADD), [d0, d1], [out])

        banks = [Reg(st.enter_context(nc.psum_tensor(f"ps{i}", [128, 512], F32))[:], f"ps{i}", psum=True) for i in range(8)]
        P = PsumPool(banks)

        cst = R([128, 6, 512], F32, "cst")
        S.dma("sp", cst, d_cst[:, 1:7, :])
        ident_bf = R([128, 128], BF16, "ident")
        bd_bf = R([128, 128], BF16, "bd")
        ones_bf = R([128, 128], BF16, "ones")
        S.dma("pool", ident_bf, d_cst[:, 0, 0:128])
        S.dma("pool", bd_bf, d_cst[:, 0, 128:256])
        memset("dve", ones_bf, 1.0)
        mask_si = cst[0:64, 0, :]
        mask_sl = cst[0:64, 1, :]
        mask_incl = cst[0:64, 2, :]
        identrep = cst[0:64, 3, :]
        resetm = cst[:, 4, :]
        sel4 = cst[0:4, 5, :]

        pv = R([128, DEPTH, NPV], F32, "pv")
        S.dma("sp", pv, d_pv)
        pvd = R([128, DEPTH, 24], F32, "pvd")
        for l in range(DEPTH):
            ts("dve", pvd[:, l, 0:15], pv[:, l, PV_MU:PV_MU + 15], -1.0, 1.0, MULT, ADD)
            ts("dve", pvd[:, l, 15:19], pv[:, l, PV_KA:PV_KA + 4], -1.0, 1.0, MULT, ADD)
            ts("dve", pvd[:, l, 19:20], pv[:, l, PV_FB:PV_FB + 1], -1.0, None, MULT)

        def pcol(l, c):
            return pv[:, l, c:c + 1]

        h_t = sb([128, 8, SEG], F32, "h")
        h = [[Reg(h_t[:, c, tb * 512:(tb + 1) * 512]) for tb in range(NTB)] for c in range(8)]
        xn_t = sb([128, 8, SEG], BF16, "xn")
        xn = [[Reg(xn_t[:, c, tb * 512:(tb + 1) * 512]) for tb in range(NTB)] for c in range(8)]
        ya_t = sb([128, 4, SEG], BF16, "ya")
        ya = [[Reg(ya_t[:, c, tb * 512:(tb + 1) * 512]) for tb in range(NTB)] for c in range(4)]
        yb_t = sb([128, 4, SEG], BF16, "yb")
        yb = [[Reg(yb_t[:, c, tb * 512:(tb + 1) * 512]) for tb in range(NTB)] for c in range(4)]
        u_t = sb([128, 8, SEG], BF16, "u")
        u = [[Reg(u_t[:, c, tb * 512:(tb + 1) * 512]) for tb in range(NTB)] for c in range(8)]
        vf_t = sb([128, 4, SEG], BF16, "vfirst")
        vfirst = [[Reg(vf_t[:, c, tb * 512:(tb + 1) * 512]) for tb in range(NTB)] for c in range(4)]
        lw_t = sb([128, 4, SEG], BF16, "lora")
        lora = [[Reg(lw_t[:, c, tb * 512:(tb + 1) * 512]) for tb in range(NTB)] for c in range(4)]

        S_t = sb([128, DEPTH, 4, 128], F32, "Sst")
        Sst = [[Reg(S_t[:, l, hp, :]) for hp in range(4)] for l in range(DEPTH)]
        CN_t = sb([128, DEPTH, 4, 256], F32, "CNst")
        CNst = [[Reg(CN_t[:, l, hd, :]) for hd in range(4)] for l in range(DEPTH)]
        car_t = sb([128, DEPTH, 16], F32, "car")
        car = [[Reg(car_t[:, l, ci:ci + 1]) for ci in range(15)] for l in range(DEPTH)]
        cqk_t = sb([128, DEPTH, 8, 3], F32, "cqk")
        cqk = [[Reg(cqk_t[:, l, c, :]) for c in range(8)] for l in range(DEPTH)]
        state_all = Reg(S_t, "Sall"), Reg(CN_t, "CNall"), Reg(car_t, "carall"), Reg(cqk_t, "cqkall")

        NW = 3
        WSZ = 4096
        wring = [R([128, WSZ], BF16, f"wslot{i}") for i in range(NW)]
        wrr = [0]

        def wslot():
            r = wring[wrr[0] % NW]
            wrr[0] += 1
            return r

        WK = {"f1i": (22, 2048), "f1o": (8, 2816), "f2i": (22, 2048), "f2o": (8, 2816),
              "sh": (1, 2560), "rw": (4, 3072), "ml": (4, 4096), "mg": (8, 3072), "wo": (8, 1024)}
        scr = {}
        if cfg.prepass:
            for k_, (n_, c_) in WK.items():
                t_ = nc.dram_tensor("scr_" + k_, [DEPTH, n_, 128, c_], BF16, kind="Internal").ap()
                scr[k_] = [[Reg(t_[l, i], f"scr_{k_}_{l}_{i}") for i in range(n_)] for l in range(DEPTH)]

        def k8(slot, n):
            return slot.v(slot.ap[:, 0:8 * n].rearrange("p (k n) -> p k n", k=8))

        def fill_f32(kind, slot, l, i):
            def cols(wv, dst0, dsrc, col0, n, first):
                S.dma("pool", wv[:, :, dst0:dst0 + n], dsrc[l, :, col0:col0 + n].rearrange("(k p) n -> p k n", p=128),
                      join=not first)
            if kind in ("f1i", "f2i"):
                dw = d_ffn1_in if kind == "f1i" else d_ffn2_in
                wv = k8(slot, 256)
                cols(wv, 0, dw, i * 128, 128, True)
                cols(wv, 128, dw, DFF + i * 128, 128, False)
            elif kind in ("f1o", "f2o"):
                dw = d_ffn1_out if kind == "f1o" else d_ffn2_out
                wv = slot.v(slot.ap[:, 0:22 * 128].rearrange("p (j n) -> p j n", j=22))
                S.dma("pool", wv, dw[l, :, i * 128:(i + 1) * 128].rearrange("(j p) n -> p j n", p=128))
            elif kind == "sh":
                wv = k8(slot, 320)
                cols(wv, 0, d_win, 1536, 128, True)
                cols(wv, 128, d_win, 1664, 160, False)
                if l > 0:
                    S.dma("pool", wv[:, :, 288:320], d_vd[l - 1].rearrange("(k p) n -> p k n", p=128), join=True)
            elif kind == "rw":
                wv = k8(slot, 384)
                cols(wv, 0, d_win, i * 128, 128, True)
                cols(wv, 128, d_win, 512 + i * 128, 128, False)
                cols(wv, 256, d_win, 1024 + i * 128, 128, False)
            elif kind == "ml":
                wv = k8(slot, 512)
                cols(wv, 0, d_win, 1824 + i * 128, 128, True)
                cols(wv, 128, d_win, 2336 + i * 128, 128, False)
                cols(wv, 256, d_win, 2848 + i * 128, 128, False)
                cols(wv, 384, d_win, 3360 + i * 128, 128, False)
            elif kind == "mg":
                bra = slot.v(slot.ap[:, 0:512].rearrange("p (k n) -> p k n", k=4))
                brb = slot.v(slot.ap[:, 512:1024].rearrange("p (k n) -> p k n", k=4))
                ga = slot.v(slot.ap[:, 1024:2048].rearrange("p (k n) -> p k n", k=8))
                gb = slot.v(slot.ap[:, 2048:3072].rearrange("p (k n) -> p k n", k=8))
                S.dma("pool", bra, d_bra[l, :, i * 128:(i + 1) * 128].rearrange("(k p) n -> p k n", p=128))
                S.dma("pool", brb, d_brb[l, :, i * 128:(i + 1) * 128].rearrange("(k p) n -> p k n", p=128), join=True)
                S.dma("pool", ga, d_win[l, :, 3880 + i * 128:3880 + (i + 1) * 128].rearrange("(k p) n -> p k n", p=128), join=True)
                S.dma("pool", gb, d_win[l, :, 4904 + i * 128:4904 + (i + 1) * 128].rearrange("(k p) n -> p k n", p=128), join=True)
            elif kind == "wo":
                wv = k8(slot, 128)
                S.dma("pool", wv, d_wout[l, :, i * 128:(i + 1) * 128].rearrange("(k p) n -> p k n", p=128))

        def wtile(kind, l, i):
            slot = wslot()
            if cfg.prepass:
                S.dma("sp", slot[:, 0:WK[kind][1]], scr[kind][l][i])
            else:
                fill_f32(kind, slot, l, i)
            return slot

        def prepass():
            for l in range(DEPTH):
                for kind, (n_, c_) in WK.items():
                    for i in range(n_):
                        slot = wslot()
                        fill_f32(kind, slot, l, i)
                        S.dma("sp", scr[kind][l][i], slot[:, 0:c_])

        WAw = R([128, 512], BF16, "WAw")
        WAa = R([128, 512], BF16, "WAa")
        G0w = R([128, 512], BF16, "G0w")
        G1w = R([32, 512], BF16, "G1w")
        VUw = R([32, 512], BF16, "VUw")
        memset("dve", WAw, 0.0)
        memset("dve", WAa, 0.0)
        Wg = R([128, 8, 8], BF16, "Wg")

        HID_COLS = 22 * SEG
        ar_used = [0]
        ARENA_F32 = 22032
        arena = sb([128, ARENA_F32], F32, "arena")
        mix_regs = []

        def AR(parts, cols, dt=F32, name=""):
            ncol32 = cols if dt == F32 else (cols + 1) // 2
            a = arena[0:parts, ar_used[0]:ar_used[0] + ncol32]
            ar_used[0] += ncol32
            assert ar_used[0] <= ARENA_F32, "arena overflow"
            if dt != F32:
                a = a.bitcast(BF16)
            r = Reg(a, name)
            mix_regs.append(r)
            return r

        NF = 15
        Fw = [AR(128, 512, F32, f"F{i}") for i in range(3)]
        Bw = [AR(128, 512, BF16, f"B{i}") for i in range(2)]
        shared_regs = list(mix_regs)
        hid_off = ar_used[0]
        hid_bf = arena[:, hid_off:hid_off + HID_COLS // 2].bitcast(BF16)
        hid = [[Reg(hid_bf[:, (j * NTB + tb) * 512:(j * NTB + tb + 1) * 512]) for tb in range(NTB)] for j in range(22)]
        mix_regs = []
        Fw += [AR(128, 512, F32, f"F{i}") for i in range(3, NF)]
        LT = [AR(128, 512, F32, f"LT{i}") for i in range(2)]
        Bw += [AR(128, 512, BF16, f"B{i}") for i in range(2, 8)]
        ARpA = AR(128, 1024, BF16, "ARpA")
        ARpB = AR(128, 1024, BF16, "ARpB")
        vpad = AR(64, 2048, BF16, "vpad")
        Btp = AR(64, 2048, BF16, "Btp")
        Ktp = AR(64, 2048, BF16, "Ktp")
        Nb = [AR(64, 1024, BF16, f"Nb{h_}") for h_ in range(2)]
        Nk = [AR(64, 1024, BF16, f"Nk{h_}") for h_ in range(2)]
        Qb = [AR(64, 512, BF16, f"Qb{i}") for i in range(2)]
        Pb = [AR(64, 512, BF16, f"Pb{i}") for i in range(2)]
        Tq = [AR(64, 512, BF16, f"Tq{i}") for i in range(2)]
        Xs = AR(64, 128, BF16, "Xs")
        Unp = AR(64, 256, BF16, "Unp")
        Sbf = AR(128, 128, BF16, "Sbf")
        SPt = AR(128, 128, F32, "SPt")
        CNbf = AR(128, 256, BF16, "CNbf")
        CNe = AR(128, 256, F32, "CNe")
        zq = AR(128, 516, F32, "zq")
        zk = AR(128, 516, F32, "zk")
        vt1 = AR(64, 2048, BF16, "vt1")
        kto = AR(64, 1024, BF16, "kto")
        sTm = AR(64, 512, BF16, "sTm")
        Gg = [Fw[9 + i][0:4, :] for i in range(4)]
        hid_regs = [r for row in hid for r in row]

        def zero_pads():
            memset("dve", ARpA, 0.0)
            memset("dve", ARpB, 0.0)
            memset("dve", vpad, 0.0)
            memset("dve", Btp, 0.0)
            memset("dve", Ktp, 0.0)
            memset("dve", Unp, 0.0)
            memset("dve", vt1, 1.0)

        def to_mixer():
            S.transfer_hazards(hid_regs, mix_regs)
            zero_pads()

        def to_ffn():
            S.transfer_hazards(mix_regs, hid_regs)

        def zero_pads_unused():
            memset("dve", ARpA, 0.0)
            memset("dve", ARpB, 0.0)
            memset("dve", vpad, 0.0)
            memset("dve", Btp, 0.0)
            memset("dve", Ktp, 0.0)
            memset("dve", Unp, 0.0)
            memset("dve", vt1, 1.0)

        def rmsnorm(l, gbase, tb, dst=None):
            ps = P.alloc()
            for c in range(8):
                sq = Bw[c % 2] if dst is None else Bw[c % 2]
                act(sq, h[c][tb], AF.Square)
                mm(ps, ones_bf, sq, c == 0, c == 7)
            rstd = Fw[2]
            act(rstd, ps, AF.Sqrt, bias=1e-6, scale=1.0 / D)
            P.release(ps)
            recip(rstd, rstd)
            for c in range(8):
                o = xn[c][tb] if dst is None else h[c][tb]
                stt("dve", o, h[c][tb], pcol(l, gbase + c), rstd, MULT, MULT)

        def ffn(l, which, gbase):
            for tb in range(NTB):
                rmsnorm(l, gbase, tb)
            for j in range(22):
                wt = wtile("f%di" % which, l, j)
                wv = k8(wt, 256)
                for tb in range(NTB):
                    pg = P.alloc()
                    pu = P.alloc()
                    for k in range(8):
                        mm(pg, wv[:, k, 0:128], xn[k][tb], k == 0, k == 7)
                    for k in range(8):
                        mm(pu, wv[:, k, 128:256], xn[k][tb], k == 0, k == 7)
                    sg = Fw[j % 2]
                    act(sg, pg, AF.Silu)
                    tt("dve", hid[j][tb], sg, pu, MULT)
                    P.release(pg)
                    P.release(pu)
            for c in range(8):
                wt = wtile("f%do" % which, l, c)
                wv = wt.v(wt.ap[:, 0:22 * 128].rearrange("p (j n) -> p j n", j=22))
                for tb in range(NTB):
                    ps = P.alloc()
                    for j in range(22):
                        mm(ps, wv[:, j, :], hid[j][tb], j == 0, j == 21)
                    stt("dve", h[c][tb], ps, 0.5, h[c][tb], MULT, ADD)
                    P.release(ps)

        lt_rr = [0]

        def lerp(ps, rows, l, ci, out):
            tmp = LT[lt_rr[0] % 2]
            lt_rr[0] += 1
            mu = pv[0:rows, l, PV_MU + ci:PV_MU + ci + 1]
            omu = pvd[0:rows, l, ci:ci + 1]
            cr = car[l][ci]
            act(tmp[0:rows, 1:512], ps[0:rows, 0:511], AF.Copy, scale=mu)
            act(tmp[0:rows, 0:1], cr[0:rows, :], AF.Copy, scale=mu)
            stt("dve", out, ps[0:rows, :], omu, tmp[0:rows, :], MULT, ADD)
            act(cr[0:rows, :], ps[0:rows, 511:512], AF.Copy)

        def proj(wv, c0, c1, tb, rows=128):
            ps = P.alloc()
            for k in range(8):
                mm(ps[0:rows, :], wv[:, k, c0:c1], xn[k][tb], k == 0, k == 7)
            return ps

        def load_cols(wv, dst0, dsrc, l, col0, n, first):
            S.dma("pool", wv[:, :, dst0:dst0 + n], dsrc[l, :, col0:col0 + n].rearrange("(k p) n -> p k n", p=128),
                  join=not first)

        def mixer(l):
            for tb in range(NTB):
                rmsnorm(l, PV_MIX, tb)
            S.dma("pool", WAw[0:64, :], d_wup[l])
            S.dma("pool", WAa[64:128, :], d_aup[l])
            S.dma("pool", G0w, d_gup[l, 0:128, :])
            S.dma("pool", G1w, d_gup[l, 128:160, :])
            if l > 0:
                S.dma("pool", VUw, d_vu[l - 1])
            wt = wtile("sh", l, 0)
            wv = k8(wt, 320)
            S.dma("pool", Wg, d_win[l, :, 3872:3880].rearrange("(k p) n -> p k n", p=128))
            for tb in range(NTB):
                if cfg.do_rw:
                    ps = proj(wv, 0, 128, tb)
                    t_ = Fw[0]
                    lerp(ps, 128, l, 12, t_)
                    P.release(ps)
                    act(lora[0][tb][0:64, :], t_[0:64, :], AF.Tanh)
                    cp("act", lora[0][tb][64:128, :], t_[64:128, :])
                    ps = proj(wv, 128, 256, tb)
                    lerp(ps, 128, l, 13, t_)
                    P.release(ps)
                    act(lora[1][tb], t_, AF.Sigmoid)
                    ps = proj(wv, 256, 288, tb, rows=32)
                    lerp(ps, 32, l, 14, t_[0:32, :])
                    P.release(ps)
                    act(lora[2][tb][0:32, :], t_[0:32, :], AF.Sigmoid)
                    if l > 0:
                        ps = proj(wv, 288, 320, tb, rows=32)
                        cp("act", lora[3][tb][0:32, :], ps[0:32, :])
                        P.release(ps)
            if cfg.do_rw:
                for hp in range(4):
                    rwkv_pair(l, hp)
            else:
                for hp in range(4):
                    for tb in range(NTB):
                        memset("dve", ya[hp][tb], 0.0)
            if cfg.do_ml:
                for hd in range(4):
                    mlstm_head(l, hd)
            else:
                for hd in range(4):
                    for tb in range(NTB):
                        memset("dve", yb[hd][tb], 0.0)
            for oc in range(8):
                wt2 = wtile("mg", l, oc)
                bra = wt2.v(wt2.ap[:, 0:512].rearrange("p (k n) -> p k n", k=4))
                brb = wt2.v(wt2.ap[:, 512:1024].rearrange("p (k n) -> p k n", k=4))
                ga = wt2.v(wt2.ap[:, 1024:2048].rearrange("p (k n) -> p k n", k=8))
                gb = wt2.v(wt2.ap[:, 2048:3072].rearrange("p (k n) -> p k n", k=8))
                for tb in range(NTB):
                    pa = P.alloc()
                    for k in range(4):
                        mm(pa, bra[:, k, :], ya[k][tb], k == 0, k == 3)
                    pb = P.alloc()
                    for k in range(4):
                        mm(pb, brb[:, k, :], yb[k][tb], k == 0, k == 3)
                    pga = P.alloc()
                    for k in range(8):
                        mm(pga, ga[:, k, :], xn[k][tb], k == 0, k == 7)
                    t1, t2 = Fw[0], Fw[1]
                    act(t1, pga, AF.Sigmoid)
                    P.release(pga)
                    pgb = P.alloc()
                    for k in range(8):
                        mm(pgb, gb[:, k, :], xn[k][tb], k == 0, k == 7)
                    act(t2, pgb, AF.Sigmoid)
                    P.release(pgb)
                    tt("dve", t1, t1, pa, MULT)
                    tt("dve", t2, t2, pb, MULT)
                    tt("dve", u[oc][tb], t1, t2, ADD)
                    P.release(pa)
                    P.release(pb)
            for oc in range(8):
                wt2 = wtile("wo", l, oc)
                wo = k8(wt2, 128)
                for tb in range(NTB):
                    ps = P.alloc()
                    for k in range(8):
                        mm(ps, wo[:, k, :], u[k][tb], k == 0, k == 7)
                    tt("dve", h[oc][tb], h[oc][tb], ps, ADD)
                    P.release(ps)

        def rwkv_pair(l, hp):
            wt = wtile("rw", l, hp)
            wv = k8(wt, 384)
            Rr, Kk, Vv, SGW, Aa, KK, T1, T2, CUM, EP, EM, EX, BON, Gt, Yt = Fw
            Rt, Kt, Bt, At, Vbf, Tb1, Tb2, _ = Bw
            ARp = (ARpA, ARpB)
            csl = slice(hp * 128, (hp + 1) * 128)
            for tb in range(NTB):
                for idx, (dst, ci) in enumerate(((Rr, hp), (Kk, 4 + hp), (Vv, 8 + hp))):
                    ps = proj(wv, idx * 128, (idx + 1) * 128, tb)
                    lerp(ps, 128, l, ci, dst)
                    P.release(ps)
                ps = P.alloc()
                mm(ps, WAw[:, csl], lora[0][tb])
                act(SGW, ps, AF.Sigmoid, bias=pcol(l, PV_W0 + hp))
                P.release(ps)
                ps = P.alloc()
                mm(ps, WAa[:, csl], lora[0][tb])
                act(Aa, ps, AF.Sigmoid, bias=pcol(l, PV_A0 + hp))
                P.release(ps)
                ps = P.alloc()
                mm(ps, G0w[:, csl], lora[1][tb], True, False)
                mm(ps, G1w[0:32, csl], lora[2][tb][0:32, :], False, True)
                cp("act", Gt, ps)
                P.release(ps)
                if l > 0:
                    ps = P.alloc()
                    mm(ps, VUw[0:32, csl], lora[3][tb][0:32, :])
                    act(T1, ps, AF.Sigmoid, bias=pcol(l, PV_VB + hp))
                    P.release(ps)
                    tt("dve", T2, vfirst[hp][tb], Vv, SUB)
                    tt("dve", T2, T2, T1, MULT)
                    tt("dve", Vv, Vv, T2, ADD)
                else:
                    cp("act", vfirst[hp][tb], Vv)
                ts("dve", KK, Kk, pcol(l, PV_KK + hp), None, MULT)
                tt("dve", Tb1, KK, KK, MULT)
                ps = P.alloc()
                mm(ps, bd_bf, Tb1)
                act(T1, ps, AF.Sqrt)
                P.release(ps)
                ts("dve", T1, T1, 1e-12, None, MAXOP)
                recip(T1, T1)
                tt("dve", KK, KK, T1, MULT)
                ts("dve", T1, Aa, pcol(l, PV_KA + hp), pvd[:, l, 15 + hp:16 + hp], MULT, ADD)
                tt("dve", Kk, Kk, T1, MULT)
                stt("dve", Tb2, Rr, pcol(l, PV_RK + hp), Kk, MULT, MULT)
                ps = P.alloc()
                mm(ps, bd_bf, Tb2)
                tt("dve", BON, ps, Vv, MULT)
                P.release(ps)
                scan(CUM, resetm, SGW)
                act(EP, CUM, AF.Exp, scale=-C0)
                act(EM, CUM, AF.Exp, scale=C0)
                tt("dve", T1, CUM, SGW, SUB)
                act(EX, T1, AF.Exp, scale=-C0)
                tt("dve", Rt, Rr, EP, MULT)
                tt("dve", Kt, Kk, EM, MULT)
                tt("dve", T1, KK, Aa, MULT)
                tt("dve", Bt, T1, EM, MULT)
                tt("dve", At, KK, EX, MULT)
                cp("act", Vbf, Vv)
                for h_ in range(2):
                    psl = slice(64 * h_, 64 * h_ + 64)
                    av = ARp[h_].v(ARp[h_].ap.rearrange("p (c two t) -> p c two t", c=8, two=2))
                    cp("act", av[psl, :, 0, :], At.v(At.ap.rearrange("p (c t) -> p c t", c=8))[psl])
                    cp("act", av[psl, :, 1, :], Rt.v(Rt.ap.rearrange("p (c t) -> p c t", c=8))[psl])
                for src, dstp in ((Vbf, vpad), (Bt, Btp), (Kt, Ktp)):
                    dv = dstp.v(dstp.ap.rearrange("p (c two n) -> p c two n", c=8, two=2))
                    for q in range(2):
                        ps = P.alloc()
                        for cc in range(4):
                            c = q * 4 + cc
                            mm(ps[0:64, cc * 128:(cc + 1) * 128], src[:, c * 64:(c + 1) * 64], ident_bf)
                        psv = ps.v(ps.ap[0:64, :].rearrange("p (c n) -> p c n", c=4))
                        cp("act", dv[:, q * 4:q * 4 + 4, 0, 0:64], psv[:, :, 0:64])
                        cp("dve", dv[:, q * 4:q * 4 + 4, 1, 64:128], psv[:, :, 64:128])
                        P.release(ps)
                NbV = [x.v(x.ap.rearrange("p (c n) -> p c n", c=8)) for x in Nb]
                NkV = [x.v(x.ap.rearrange("p (c n) -> p c n", c=8)) for x in Nk]
                ARV = [x.v(x.ap.rearrange("p (c n) -> p c n", c=8)) for x in ARp]
                idv = identrep.rearrange("p (c t) -> p c t", c=8)
                for q in range(2):
                    for h_ in range(2):
                        psb = P.alloc()
                        psk = P.alloc()
                        for cc in range(4):
                            c = q * 4 + cc
                            mm(psb[0:64, cc * 128:(cc + 1) * 128], Bt[:, c * 64:(c + 1) * 64], ARV[h_][:, c, :])
                            mm(psk[0:64, cc * 128:(cc + 1) * 128], Kt[:, c * 64:(c + 1) * 64], ARV[h_][:, c, :])
                        tt("dve", Nb[h_][:, q * 512:(q + 1) * 512], psb[0:64, :], mask_si, MULT)
                        tt("dve", Nk[h_][:, q * 512:(q + 1) * 512], psk[0:64, :], mask_si, MULT)
                        P.release(psb)
                        P.release(psk)
                    pst = P.alloc()
                    for h_ in range(2):
                        for cc in range(4):
                            c = q * 4 + cc
                            i8 = h_ * 4 + cc
                            mm(pst[0:64, i8 * 64:(i8 + 1) * 64], ARV[h_][:, c, 0:64], Bt[:, c * 64:(c + 1) * 64])
                    tt("dve", Qb[0], pst[0:64, :], mask_sl, MULT)
                    P.release(pst)
                    Tt = Tq[q]
                    TtV = Tt.v(Tt.ap.rearrange("p (h c t) -> p h c t", h=2, c=4))
                    for h_ in range(2):
                        tt("dve", TtV[:, h_, :, :], idv[:, 0:4, :], NbV[h_][:, q * 4:q * 4 + 4, 0:64], SUB)

                    def blk(x, i8):
                        return x[:, i8 * 64:(i8 + 1) * 64]

                    Pm = lambda i8: NbV[i8 // 4][:, q * 4 + i8 % 4, 0:64]
                    Qm = lambda i8: blk(Qb[0], i8)
                    for lev in range(5):
                        newQ = Qb[(lev + 1) % 2]
                        psq = P.alloc()
                        for i8 in range(8):
                            mm(blk(psq[0:64, :], i8), Pm(i8), Qm(i8))
                        cp("act", newQ, psq[0:64, :])
                        P.release(psq)
                        if lev < 4:
                            newP = Pb[lev % 2]
                            psp = P.alloc()
                            for i8 in range(8):
                                mm(blk(psp[0:64, :], i8), Qm(i8), Pm(i8))
                            cp("act", newP, psp[0:64, :])
                            P.release(psp)
                        pt2 = P.alloc()
                        for i8 in range(8):
                            mm(blk(pt2[0:64, :], i8), blk(newQ, i8), blk(Tt, i8))
                        tt("dve", Tt, Tt, pt2[0:64, :], ADD)
                        P.release(pt2)
                        if lev < 4:
                            Pm = (lambda np_: (lambda i8: blk(np_, i8)))(newP)
                        Qm = (lambda nq_: (lambda i8: blk(nq_, i8)))(newQ)
                Sreg = Sst[l][hp]
                cp("act", Sbf, Sreg)
                vpv = vpad.v(vpad.ap.rearrange("p (c two n) -> p c two n", c=8, two=2))
                bpv = Btp.v(Btp.ap.rearrange("p (c two n) -> p c two n", c=8, two=2))
                kpv = Ktp.v(Ktp.ap.rearrange("p (c two n) -> p c two n", c=8, two=2))
                unv = Unp.v(Unp.ap.rearrange("p (two n) -> p two n", two=2))
                un4 = Unp.v(Unp.ap.rearrange("p (b n) -> p b n", b=4))
                psY = P.alloc()
                for c in range(8):
                    q, cc = c // 4, c % 4
                    ck = slice(c * 64, (c + 1) * 64)
                    psX = P.alloc()
                    mm(psX[0:64, 0:128], At[:, ck], Sbf, True, False)
                    mm(psX[0:64, 0:128], NkV[0][:, c, 0:64], vpv[:, c, 0, :], False, False)
                    mm(psX[0:64, 0:128], NkV[1][:, c, 0:64], vpv[:, c, 1, :], False, True)
                    cp("act", Xs, psX[0:64, 0:128])
                    P.release(psX)
                    psU = P.alloc()
                    mm(psU[0:64, 0:64], blk(Tq[q], cc), Xs[:, 0:64])
                    mm(psU[0:64, 64:128], blk(Tq[q], 4 + cc), Xs[:, 64:128])
                    ts("dve", un4[:, 0:4:3, :], psU.v(psU.ap[0:64, 0:128].rearrange("p (two n) -> p two n", two=2)),
                       -1.0, None, MULT)
                    P.release(psU)
                    mm(psY[:, ck], Sbf, Rt[:, ck], True, False)
                    mm(psY[:, ck], unv[:, 0, :], NbV[0][:, c, 64:128], False, False)
                    mm(psY[:, ck], unv[:, 1, :], NbV[1][:, c, 64:128], False, False)
                    mm(psY[:, ck], vpv[:, c, 0, :], NkV[0][:, c, 64:128], False, False)
                    mm(psY[:, ck], vpv[:, c, 1, :], NkV[1][:, c, 64:128], False, True)
                    psS = P.alloc()
                    mm(psS[:, 0:128], bpv[:, c, 0, :], unv[:, 0, :], True, False)
                    mm(psS[:, 0:128], bpv[:, c, 1, :], unv[:, 1, :], False, False)
                    mm(psS[:, 0:128], kpv[:, c, 0, :], vpv[:, c, 0, :], False, False)
                    mm(psS[:, 0:128], kpv[:, c, 1, :], vpv[:, c, 1, :], False, True)
                    pL = EP[:, c * 64 + 63:c * 64 + 64]
                    ts("dve", SPt, Sreg, pL, None, MULT)
                    stt("dve", Sreg, psS[:, 0:128], pL, SPt, MULT, ADD)
                    P.release(psS)
                    cp("act", Sbf, Sreg)
                cp("act", Yt, psY)
                P.release(psY)
                cp("act", Tb1, Yt)
                tt("dve", Tb2, Yt, Yt, MULT)
                ps1 = P.alloc()
                mm(ps1, bd_bf, Tb1)
                ps2 = P.alloc()
                mm(ps2, bd_bf, Tb2)
                ts("dve", T1, ps1, 1.0 / 64, None, MULT)
                tt("dve", T2, T1, T1, MULT)
                stt("dve", T2, ps2, 1.0 / 64, T2, MULT, SUB)
                P.release(ps1)
                P.release(ps2)
                act(T2, T2, AF.Sqrt, bias=64e-5, scale=1.0)
                recip(T2, T2)
                tt("dve", Yt, Yt, T1, SUB)
                tt("dve", Yt, Yt, T2, MULT)
                ts("dve", Yt, Yt, pcol(l, PV_GNW + hp), pcol(l, PV_GNB + hp), MULT, ADD)
                tt("dve", Yt, Yt, BON, ADD)
                tt("dve", ya[hp][tb], Yt, Gt, MULT)

        def mlstm_head(l, hd):
            LI, LF, BC, G1 = Gg
            GT = LF
            wt = wtile("ml", l, hd)
            wv = k8(wt, 512)
            Qc, Kc, SO, ALPHA, BETA, HH, T1, T2, MEAN = Fw[0:9]
            QT, KT, Vbf, Tb1, Tb2 = Bw[0:5]
            CN = CNst[l][hd]
            for tb in range(NTB):
                if hd == 0 or NTB > 1:
                    ps = proj(Wg, 0, 4, tb, rows=4)
                    ts("dve", LI, ps[0:4, :], pv[0:4, l, PV_IB:PV_IB + 1], None, ADD)
                    P.release(ps)
                    ps = proj(Wg, 4, 8, tb, rows=4)
                    act(GT, ps[0:4, :], AF.Exp, bias=pvd[0:4, l, 19:20], scale=-1.0)
                    P.release(ps)
                    act(GT, GT, AF.Ln, bias=1.0)
                    ts("dve", LF, GT, -1.0, None, MULT)
                    scan(BC, resetm[0:4, :], LF)
                    tt("dve", G1, LI, BC, SUB)
                for (zz, dst, idx) in ((zq, Qc, 0), (zk, Kc, 1)):
                    ci = idx * 4 + hd
                    ps = proj(wv, idx * 128, (idx + 1) * 128, tb)
                    cp("act", zz[:, 3:515], ps)
                    P.release(ps)
                    cp("act", zz[:, 0:3], cqk[l][ci])
                    ts("dve", dst, zz[:, 0:512], pcol(l, PV_CW + ci), pcol(l, PV_CB + ci), MULT, ADD)
                    for j in range(1, 4):
                        stt("dve", dst, zz[:, j:j + 512], pcol(l, PV_CW + 8 * j + ci), dst, MULT, ADD)
                    cp("act", cqk[l][ci], zz[:, 512:515])
                    act(dst, dst, AF.Silu)
                ps = proj(wv, 256, 384, tb)
                cp("act", Vbf, ps)
                P.release(ps)
                ps = proj(wv, 384, 512, tb)
                act(SO, ps, AF.Sigmoid)
                P.release(ps)
                ps = P.alloc()
                mm(ps, sel4[:, hd * 128:(hd + 1) * 128], BC)
                act(ALPHA, ps, AF.Exp)
                P.release(ps)
                ps = P.alloc()
                mm(ps, sel4[:, hd * 128:(hd + 1) * 128], G1)
                act(BETA, ps, AF.Exp)
                P.release(ps)
                stt("dve", QT, Qc, 128.0 ** -0.5, ALPHA, MULT, MULT)
                tt("dve", KT, Kc, BETA, MULT)
                v1 = vt1.v(vt1.ap.rearrange("p (c n) -> p c n", c=8))
                ktv = kto.v(kto.ap.rearrange("p (c n) -> p c n", c=8))
                for q in range(2):
                    ps = P.alloc()
                    for cc in range(4):
                        c = q * 4 + cc
                        mm(ps[0:64, cc * 128:(cc + 1) * 128], Vbf[:, c * 64:(c + 1) * 64], ident_bf)
                    cp("act", v1[:, q * 4:q * 4 + 4, 0:128], ps.v(ps.ap[0:64, :].rearrange("p (c n) -> p c n", c=4)))
                    P.release(ps)
                    ps = P.alloc()
                    for cc in range(4):
                        c = q * 4 + cc
                        mm(ps[0:64, cc * 128:(cc + 1) * 128], KT[:, c * 64:(c + 1) * 64], ident_bf)
                    cp("act", kto[:, q * 512:(q + 1) * 512], ps[0:64, :])
                    P.release(ps)
                ps = P.alloc()
                for c in range(8):
                    ck = slice(c * 64, (c + 1) * 64)
                    mm(ps[0:64, ck], KT[:, ck], QT[:, ck])
                tt("dve", sTm, ps[0:64, :], mask_incl, MULT)
                P.release(ps)
                cp("act", CNbf, CN)
                psN = P.alloc()
                psD = P.alloc()
                for c in range(8):
                    ck = slice(c * 64, (c + 1) * 64)
                    mm(psN[:, ck], v1[:, c, 0:128], sTm[:, ck], True, False)
                    mm(psN[:, ck], CNbf[:, 0:128], QT[:, ck], False, True)
                    mm(psD[:, ck], ones_bf[0:64, :], sTm[:, ck], True, False)
                    mm(psD[:, ck], CNbf[:, 128:256], QT[:, ck], False, True)
                    psC = P.alloc()
                    mm(psC[:, 0:256], ktv[:, c, :], v1[:, c, :])
                    ebL = ALPHA[:, c * 64 + 63:c * 64 + 64]
                    ts("dve", CNe, CN, ebL, None, MULT)
                    stt("dve", CN, psC[:, 0:256], ebL, CNe, MULT, ADD)
                    P.release(psC)
                    cp("act", CNbf, CN)
                act(T1, psD, AF.Abs)
                P.release(psD)
                ts("dve", T1, T1, 1.0, None, MAXOP)
                recip(T1, T1)
                tt("dve", HH, psN, T1, MULT)
                P.release(psN)
                cp("act", Tb1, HH)
                tt("dve", Tb2, HH, HH, MULT)
                ps1 = P.alloc()
                mm(ps1, ones_bf, Tb1)
                ps2 = P.alloc()
                mm(ps2, ones_bf, Tb2)
                ts("dve", MEAN, ps1, 1.0 / 128, None, MULT)
                tt("dve", T2, MEAN, MEAN, MULT)
                stt("dve", T2, ps2, 1.0 / 128, T2, MULT, SUB)
                P.release(ps1)
                P.release(ps2)
                act(T2, T2, AF.Sqrt, bias=1e-5, scale=1.0)
                recip(T2, T2)
                tt("dve", HH, HH, MEAN, SUB)
                tt("dve", HH, HH, T2, MULT)
                stt("dve", yb[hd][tb], HH, pcol(l, PV_NW + hd), SO, MULT, MULT)

        out_toks = []
        if cfg.prepass:
            prepass()
        for s in range(NS):
            for rg in state_all:
                memset("dve", rg, 0.0)
            for g in range(NSEG):
                t0 = g * SEG
                for c in range(8):
                    for tb in range(NTB):
                        S.dma("sp", h[c][tb], xT[s, c * 128:(c + 1) * 128, t0 + tb * 512:t0 + (tb + 1) * 512])
                for l in range(DEPTH):
                    if cfg.do_ffn:
                        to_ffn()
                        ffn(l, 1, PV_FFN1)
                    if cfg.do_mix:
                        to_mixer()
                        mixer(l)
                    if cfg.do_ffn:
                        to_ffn()
                        ffn(l, 2, PV_FFN2)
                for tb in range(NTB):
                    rmsnorm(0, PV_FIN, tb, dst="h")
                    for c in range(8):
                        out_toks.append(S.dma("sp", outT[s, c * 128:(c + 1) * 128, t0 + tb * 512:t0 + (tb + 1) * 512],
                                              h[c][tb]))
        S.wait_all_at_end("sp", [("dma", k, 16 * S.dma_cnt[k]) for k in range(S.n_dma_sems) if S.dma_cnt[k]])
        S.emit(st)
    return nc


_CACHE = {}


def run_cores(inp, cfg, n_cores):
    depth = cfg.DEPTH
    key = (cfg.NS, cfg.T, cfg.DEPTH, cfg.NTB, cfg.do_ffn, cfg.do_rw, cfg.do_ml, cfg.do_mix, cfg.prepass)
    if key not in _CACHE:
        _CACHE[key] = build_program(cfg)
    nc = _CACHE[key]
    pv = pack_params(inp, depth)
    consts = make_consts()
    def f(k):
        a = np.ascontiguousarray(inp[k], dtype=np.float32)
        if a.shape[0] == 0:
            a = np.zeros((1,) + a.shape[1:], np.float32)
        return a
    shared = {
        "ffn1_w_in": f("ffn1_w_in"), "ffn1_w_out": f("ffn1_w_out"), "ffn2_w_in": f("ffn2_w_in"),
        "ffn2_w_out": f("ffn2_w_out"), "w_in": f("w_in"), "rw_w_up": f("rw_w_up"), "rw_a_up": f("rw_a_up"),
        "rw_g_up": f("rw_g_up"), "vres_down": f("vres_down"), "vres_up": f("vres_up"), "br_a": f("br_a"),
        "br_b": f("br_b"), "w_out": f("w_out"), "pvec": pv, "consts": consts,
    }
    x = np.asarray(inp["x"], np.float32)
    in_maps = []
    for c in range(n_cores):
        xs = x[c * cfg.NS:(c + 1) * cfg.NS]
        m = dict(shared)
        m["xT"] = np.ascontiguousarray(xs.transpose(0, 2, 1))
        in_maps.append(m)
    res = run_bass_kernel_spmd(nc, in_maps, core_ids=list(range(n_cores)))
    outs = [np.asarray(r["outT"]).transpose(0, 2, 1) for r in res.results]
    return np.ascontiguousarray(np.concatenate(outs, axis=0), dtype=np.float32)


def kernel(**inputs):
    cfg = Cfg(NS=4, T=2048, DEPTH=4, NTB=1)
    return run_cores(inputs, cfg, 8)
```

```python
import numpy as np
from contextlib import ExitStack
import concourse.bass as bass
import concourse.mybir as mybir
from concourse.bass_utils import run_bass_kernel_spmd

F32 = mybir.dt.float32
BF16 = mybir.dt.bfloat16
AF = mybir.ActivationFunctionType
ALU = mybir.AluOpType
MULT, ADD, SUB, MAXOP = ALU.mult, ALU.add, ALU.subtract, ALU.max
EPOCH = 16000
ENGS = ("pe", "act", "dve", "pool", "sp")

D = 1024
DFF = 2816
NIN = 5928
C0 = float(np.exp(-0.5))


class Reg:
    __slots__ = ("ap", "w", "rs", "name", "track", "psum")

    def __init__(self, ap, name="", track=True, psum=False):
        self.ap = ap
        self.w = []
        self.rs = {}
        self.name = name
        self.track = track
        self.psum = psum

    def __getitem__(self, idx):
        return View(self, self.ap[idx])

    @property
    def reg(self):
        return self

    def v(self, ap):
        return View(self, ap)


class View:
    __slots__ = ("reg", "ap")

    def __init__(self, reg, ap):
        self.reg = reg
        self.ap = ap

    def __getitem__(self, idx):
        return View(self.reg, self.ap[idx])

    def rearrange(self, *a, **k):
        return View(self.reg, self.ap.rearrange(*a, **k))

    def bitcast(self, dt):
        return View(self.reg, self.ap.bitcast(dt))


def tok_key(tok):
    return tok[0] if tok[0] in ENGS else ("d", tok[1])


class Sched:
    def __init__(self, nc, n_dma_sems=32, same_engine_sync=True):
        self.nc = nc
        self.ops = {e: [] for e in ENGS}
        self.count = {e: 0 for e in ENGS}
        self.waited = {e: {} for e in ENGS}
        self.n_dma_sems = n_dma_sems
        self.dma_cnt = [0] * n_dma_sems
        self.dma_rr = 0
        self.same_engine_sync = same_engine_sync
        self.needed = {e: set() for e in ENGS}
        self.final_waits = []

    def _need(self, eng, tok, waits):
        if tok is None:
            return
        key = tok_key(tok)
        val = tok[-1]
        if key == eng and (eng == "pe" or not self.same_engine_sync):
            return
        if self.waited[eng].get(key, -1) >= val:
            return
        if key in waits and waits[key][-1] >= val:
            return
        waits[key] = tok

    def _deps(self, eng, reads, writes, waits=None):
        waits = {} if waits is None else waits
        for r in reads:
            if r.reg.track:
                for t in r.reg.w:
                    self._need(eng, t, waits)
                if r.reg.psum:
                    for k_, t in r.reg.rs.items():
                        if k_ != eng:
                            self._need(eng, t, waits)
        for w in writes:
            if w.reg.track:
                for t in w.reg.w:
                    self._need(eng, t, waits)
                for t in w.reg.rs.values():
                    self._need(eng, t, waits)
        for key, tok in waits.items():
            self.waited[eng][key] = tok[-1]
            if tok[0] in ENGS:
                self.needed[tok[0]].add(tok[1])
        return list(waits.values())

    def _commit(self, tok, reads, writes, append_w=False):
        for r in reads:
            if r.reg.track:
                r.reg.rs[tok_key(tok)] = tok
        for w in writes:
            if w.reg.track:
                if append_w:
                    w.reg.w = w.reg.w + [tok]
                else:
                    w.reg.w = [tok]
                w.reg.rs = {}

    def op(self, eng, fn, reads=(), writes=()):
        waits = self._deps(eng, reads, writes)
        idx = self.count[eng]
        self.count[eng] += 1
        tok = (eng, idx)
        self.ops[eng].append((waits, fn, tok, "c"))
        self._commit(tok, reads, writes)
        return tok

    def dma(self, eng, out, in_, join=False):
        k = self.dma_rr
        self.dma_rr = (self.dma_rr + 1) % self.n_dma_sems
        waits = {}
        if self.dma_cnt[k]:
            self._need(eng, ("dma", k, 16 * self.dma_cnt[k]), waits)
        reads, writes = [in_], [out]
        if join:
            saved = out.reg.w
            out.reg.w = []
            allw = self._deps(eng, reads, writes, waits)
            out.reg.w = saved
        else:
            allw = self._deps(eng, reads, writes, waits)
        self.dma_cnt[k] += 1
        tok = ("dma", k, 16 * self.dma_cnt[k])
        oap, iap = out.ap, in_.ap
        self.count[eng] += 1
        self.ops[eng].append((allw, lambda e: e.dma_start(out=oap, in_=iap), tok, "d"))
        self._commit(tok, reads, writes, append_w=join)
        return tok

    def transfer_hazards(self, src_regs, dst_regs):
        merged = {}
        for r in src_regs:
            for t in list(r.rs.values()) + list(r.w):
                key = tok_key(t)
                if key not in merged or merged[key][-1] < t[-1]:
                    merged[key] = t
        for d in dst_regs:
            for key, t in merged.items():
                if key not in d.rs or d.rs[key][-1] < t[-1]:
                    d.rs[key] = t

    def wait_all_at_end(self, eng, toks):
        self.final_waits.append((eng, list(toks)))

    def emit(self, st):
        nc = self.nc
        rank, sems = {}, {}
        for e in ENGS:
            ms = sorted(self.needed[e])
            rank[e] = {i: r + 1 for r, i in enumerate(ms)}
            n_ep = max(1, (len(ms) + EPOCH - 1) // EPOCH)
            sems[e] = [st.enter_context(nc.semaphore(f"s_{e}_{k}")) for k in range(n_ep)]
        dsems = [st.enter_context(nc.semaphore(f"s_dma_{k}")) for k in range(self.n_dma_sems)]

        def resolve(tok):
            if tok[0] == "dma":
                return dsems[tok[1]], tok[2]
            e, i = tok
            r = rank[e][i]
            return sems[e][(r - 1) // EPOCH], (r - 1) % EPOCH + 1

        block = st.enter_context(nc.Block())

        def make_body(e):
            def body(eng):
                for waits, fn, tok, kind in self.ops[e]:
                    ws = [resolve(w) for w in waits]
                    for s, v in ws[1:]:
                        eng.wait_ge(s, v)
                    ins = fn(eng)
                    if ws:
                        ins = ins._wait_ge(ws[0][0], ws[0][1])
                    if kind == "d":
                        ins.then_inc(dsems[tok[1]], 16)
                    elif tok[1] in rank[e]:
                        s, v = resolve(tok)
                        ins.then_inc(s, 1)
                for (fe, toks) in self.final_waits:
                    if fe == e:
                        for t in toks:
                            s, v = resolve(t)
                            eng.wait_ge(s, v)
            return body

        block.tensor(make_body("pe"))
        block.scalar(make_body("act"))
        block.vector(make_body("dve"))
        block.gpsimd(make_body("pool"))
        block.sync(make_body("sp"))


class PsumPool:
    def __init__(self, regs):
        self.free = list(regs)

    def alloc(self):
        assert self.free, "PSUM pool exhausted"
        return self.free.pop(0)

    def release(self, r):
        self.free.append(r)


PV_FFN1, PV_MIX, PV_FFN2, PV_FIN = 0, 8, 16, 24
PV_MU = 32
PV_W0, PV_A0, PV_KK, PV_KA, PV_RK, PV_GNW, PV_GNB, PV_VB = 47, 51, 55, 59, 63, 67, 71, 75
PV_CW, PV_CB, PV_IB, PV_FB, PV_NW = 79, 111, 119, 120, 121
NPV = 128


def _cols(vec, n):
    out = np.zeros((128, n), np.float32)
    v = np.asarray(vec, np.float32).reshape(-1)
    full = len(v) // 128
    if full:
        out[:, :full] = v[:full * 128].reshape(full, 128).T
    rem = len(v) - full * 128
    if rem:
        out[:rem, full] = v[full * 128:]
    return out


def pack_params(inp, depth):
    pv = np.zeros((128, depth, NPV), np.float32)
    for l in range(depth):
        p = pv[:, l]
        p[:, PV_FFN1:PV_FFN1 + 8] = _cols(inp["ffn1_norm"][l], 8)
        p[:, PV_MIX:PV_MIX + 8] = _cols(inp["mix_norm"][l], 8)
        p[:, PV_FFN2:PV_FFN2 + 8] = _cols(inp["ffn2_norm"][l], 8)
        p[:, PV_FIN:PV_FIN + 8] = _cols(inp["final_norm"], 8)
        mu = inp["shift_mu"][l]
        p[:, PV_MU:PV_MU + 12] = _cols(mu[0:1536], 12)
        p[:, PV_MU + 12:PV_MU + 13] = _cols(mu[1536:1664], 1)
        p[:, PV_MU + 13:PV_MU + 14] = _cols(mu[1664:1792], 1)
        p[:, PV_MU + 14:PV_MU + 15] = _cols(mu[1792:1824], 1)
        p[:, PV_W0:PV_W0 + 4] = _cols(inp["rw_w0"][l], 4)
        p[:, PV_A0:PV_A0 + 4] = _cols(inp["rw_a0"][l], 4)
        p[:, PV_KK:PV_KK + 4] = _cols(inp["rw_k_k"][l], 4)
        p[:, PV_KA:PV_KA + 4] = _cols(inp["rw_k_a"][l], 4)
        p[:, PV_RK:PV_RK + 4] = _cols(inp["rw_r_k"][l].reshape(-1), 4)
        p[:, PV_GNW:PV_GNW + 4] = _cols(inp["rw_gn_w"][l], 4)
        p[:, PV_GNB:PV_GNB + 4] = _cols(inp["rw_gn_b"][l], 4)
        if l > 0:
            p[:, PV_VB:PV_VB + 4] = _cols(inp["vres_bias"][l - 1], 4)
        for j in range(4):
            p[:, PV_CW + 8 * j:PV_CW + 8 * j + 8] = _cols(inp["ml_conv_w"][l][j], 8)
        p[:, PV_CB:PV_CB + 8] = _cols(inp["ml_conv_b"][l], 8)
        p[0:4, PV_IB] = inp["ml_i_bias"][l]
        p[0:4, PV_FB] = inp["ml_f_bias"][l]
        p[:, PV_NW:PV_NW + 4] = _cols(inp["ml_norm_w"][l], 4)
    return pv


def make_consts():
    c = np.zeros((128, 7, 512), np.float32)
    c[:, 0, 0:128] = np.eye(128)
    bd = np.zeros((128, 128), np.float32)
    bd[0:64, 0:64] = 1.0
    bd[64:128, 64:128] = 1.0
    c[:, 0, 128:256] = bd
    s = np.arange(64)[:, None]
    t = np.arange(64)[None, :]
    strict = (s < t).astype(np.float32)
    incl = (s <= t).astype(np.float32)
    lower = (s > t).astype(np.float32)
    c[0:64, 1] = np.tile(np.concatenate([strict, incl], axis=1), (1, 4))
    c[0:64, 2] = np.tile(lower, (1, 8))
    c[0:64, 3] = np.tile(incl, (1, 8))
    c[0:64, 4] = np.tile(np.eye(64, dtype=np.float32), (1, 8))
    r = np.ones((128, 512), np.float32)
    r[:, ::64] = 0.0
    c[:, 5] = r
    for h in range(4):
        c[h, 6, h * 128:(h + 1) * 128] = 1.0
    return c


class Cfg:
    def __init__(self, NS=4, T=2048, DEPTH=4, NTB=1, do_ffn=True, do_rw=True, do_ml=True, do_mix=True,
                 prepass=True):
        self.NS, self.T, self.DEPTH, self.NTB = NS, T, DEPTH, NTB
        self.prepass = prepass
        self.do_ffn, self.do_rw, self.do_ml, self.do_mix = do_ffn, do_rw, do_ml, do_mix


def build_program(cfg):
    NS, T, DEPTH, NTB = cfg.NS, cfg.T, cfg.DEPTH, cfg.NTB
    SEG = NTB * 512
    NSEG = T // SEG
    nc = bass.Bass("TRN2", target_bir_lowering=False)

    def din(name, shape):
        return Reg(nc.dram_tensor(name, list(shape), F32, kind="ExternalInput").ap(), name, track=False)

    xT = din("xT", [NS, D, T])
    d_ffn1_in = din("ffn1_w_in", [DEPTH, D, 2 * DFF])
    d_ffn1_out = din("ffn1_w_out", [DEPTH, DFF, D])
    d_ffn2_in = din("ffn2_w_in", [DEPTH, D, 2 * DFF])
    d_ffn2_out = din("ffn2_w_out", [DEPTH, DFF, D])
    d_win = din("w_in", [DEPTH, D, NIN])
    d_wup = din("rw_w_up", [DEPTH, 64, 512])
    d_aup = din("rw_a_up", [DEPTH, 64, 512])
    d_gup = din("rw_g_up", [DEPTH, 160, 512])
    d_vd = din("vres_down", [max(DEPTH - 1, 1), D, 32])
    d_vu = din("vres_up", [max(DEPTH - 1, 1), 32, 512])
    d_bra = din("br_a", [DEPTH, 512, D])
    d_brb = din("br_b", [DEPTH, 512, D])
    d_wout = din("w_out", [DEPTH, D, D])
    d_pv = din("pvec", [128, DEPTH, NPV])
    d_cst = din("consts", [128, 7, 512])
    outT = Reg(nc.dram_tensor("outT", [NS, D, T], F32, kind="ExternalOutput").ap(), "outT", track=False)

    S = Sched(nc)
    st = ExitStack()
    with st:
        cnt = [0]

        def sb(shape, dt=F32, name=None):
            cnt[0] += 1
            nm = name or f"t{cnt[0]}"
            return st.enter_context(nc.sbuf_tensor(nm, list(shape), dt))[:]

        def R(shape, dt=F32, name=None):
            return Reg(sb(shape, dt, name), name or "")

        def rd(x, lst):
            if hasattr(x, "reg"):
                lst.append(x)
                return x.ap
            return x

        def mm(out, lhsT, rhs, start=True, stop=True):
            o, l, r = out.ap, lhsT.ap, rhs.ap
            S.op("pe", lambda e: e.matmul(o, l, r, start=start, stop=stop), [lhsT, rhs], [out])

        def act(out, in_, func, bias=None, scale=None):
            reads = [in_]
            kw = {}
            if bias is not None:
                kw["bias"] = rd(bias, reads)
            if scale is not None:
                kw["scale"] = rd(scale, reads)
            o, i = out.ap, in_.ap
            S.op("act", lambda e: e.activation(o, i, func, **kw), reads, [out])

        def ts(eng, out, in0, s1, s2=None, op0=MULT, op1=None):
            reads = [in0]
            a1 = rd(s1, reads)
            a2 = rd(s2, reads) if s2 is not None else None
            o, i = out.ap, in0.ap
            if op1 is None:
                S.op(eng, lambda e: e.tensor_scalar(o, i, a1, None, op0), reads, [out])
            else:
                S.op(eng, lambda e: e.tensor_scalar(o, i, a1, a2, op0, op1), reads, [out])

        def tt(eng, out, in0, in1, op):
            o, a, b = out.ap, in0.ap, in1.ap
            S.op(eng, lambda e: e.tensor_tensor(o, a, b, op), [in0, in1], [out])

        def stt(eng, out, in0, scalar, in1, op0, op1):
            reads = [in0, in1]
            sc = rd(scalar, reads)
            o, a, b = out.ap, in0.ap, in1.ap
            S.op(eng, lambda e: e.scalar_tensor_tensor(o, a, sc, b, op0, op1), reads, [out])

        def cp(eng, out, in_):
            o, i = out.ap, in_.ap
            if eng == "act":
                S.op("act", lambda e: e.activation(o, i, AF.Copy), [in_], [out])
            else:
                S.op(eng, lambda e: e.tensor_copy(o, i), [in_], [out])

        def memset(eng, out, val):
            o = out.ap
            S.op(eng, lambda e: e.memset(o, val), [], [out])

        def recip(out, in_):
            o, i = out.ap, in_.ap
            S.op("dve", lambda e: e.reciprocal(o, i), [in_], [out])

        def scan(out, d0, d1):
            o, a, b = out.ap, d0.ap, d1.ap
            S.op("dve", lambda e: e.tensor_tensor_scan(o, a, b, 0.0, MULT, ADD), [d0, d1], [out])

        banks = [Reg(st.enter_context(nc.psum_tensor(f"ps{i}", [128, 512], F32))[:], f"ps{i}", psum=True) for i in range(8)]
        P = PsumPool(banks)

        cstf = R([128, 2, 512], F32, "cstf")
        S.dma("sp", cstf, d_cst[:, 5:7, :])
        cstb = R([64, 4, 512], BF16, "cstb")
        S.dma("pool", cstb, d_cst[0:64, 1:5, :])
        ident_bf = R([128, 128], BF16, "ident")
        bd_bf = R([128, 128], BF16, "bd")
        ones_bf = R([128, 128], BF16, "ones")
        S.dma("pool", ident_bf, d_cst[:, 0, 0:128])
        S.dma("pool", bd_bf, d_cst[:, 0, 128:256])
        memset("dve", ones_bf, 1.0)
        mask_si = cstb[:, 0, :]
        mask_sl = cstb[:, 1, :]
        mask_incl = cstb[:, 2, :]
        identrep = cstb[:, 3, :]
        resetm = cstf[:, 0, :]
        sel4 = cstf[0:4, 1, :]

        pv = R([128, DEPTH, NPV], F32, "pv")
        S.dma("sp", pv, d_pv)
        pvd = R([128, DEPTH, 24], F32, "pvd")
        for l in range(DEPTH):
            ts("dve", pvd[:, l, 0:15], pv[:, l, PV_MU:PV_MU + 15], -1.0, 1.0, MULT, ADD)
            ts("dve", pvd[:, l, 15:19], pv[:, l, PV_KA:PV_KA + 4], -1.0, 1.0, MULT, ADD)
            ts("dve", pvd[:, l, 19:20], pv[:, l, PV_FB:PV_FB + 1], -1.0, None, MULT)

        def pcol(l, c):
            return pv[:, l, c:c + 1]

        h_t = sb([128, 8, SEG], F32, "h")
        h = [[Reg(h_t[:, c, tb * 512:(tb + 1) * 512]) for tb in range(NTB)] for c in range(8)]
        xn_t = sb([128, 8, SEG], BF16, "xn")
        xn = [[Reg(xn_t[:, c, tb * 512:(tb + 1) * 512]) for tb in range(NTB)] for c in range(8)]
        ya_t = sb([128, 4, SEG], BF16, "ya")
        ya = [[Reg(ya_t[:, c, tb * 512:(tb + 1) * 512]) for tb in range(NTB)] for c in range(4)]
        yb_t = sb([128, 4, SEG], BF16, "yb")
        yb = [[Reg(yb_t[:, c, tb * 512:(tb + 1) * 512]) for tb in range(NTB)] for c in range(4)]
        u_t = sb([128, 8, SEG], BF16, "u")
        u = [[Reg(u_t[:, c, tb * 512:(tb + 1) * 512]) for tb in range(NTB)] for c in range(8)]
        vf_t = sb([128, 4, SEG], BF16, "vfirst")
        vfirst = [[Reg(vf_t[:, c, tb * 512:(tb + 1) * 512]) for tb in range(NTB)] for c in range(4)]
        lw_t = sb([128, 4, SEG], BF16, "lora")
        lora = [[Reg(lw_t[:, c, tb * 512:(tb + 1) * 512]) for tb in range(NTB)] for c in range(4)]

        S_t = sb([128, DEPTH, 4, 128], F32, "Sst")
        Sst = [[Reg(S_t[:, l, hp, :]) for hp in range(4)] for l in range(DEPTH)]
        CN_t = sb([128, DEPTH, 4, 256], F32, "CNst")
        CNst = [[Reg(CN_t[:, l, hd, :]) for hd in range(4)] for l in range(DEPTH)]
        car_t = sb([128, DEPTH, 16], F32, "car")
        car = [[Reg(car_t[:, l, ci:ci + 1]) for ci in range(15)] for l in range(DEPTH)]
        cqk_t = sb([128, DEPTH, 8, 3], F32, "cqk")
        cqk = [[Reg(cqk_t[:, l, c, :]) for c in range(8)] for l in range(DEPTH)]
        state_all = Reg(S_t, "Sall"), Reg(CN_t, "CNall"), Reg(car_t, "carall"), Reg(cqk_t, "cqkall")

        NW = 3
        WSZ = 4096
        wring = [R([128, WSZ], BF16, f"wslot{i}") for i in range(NW)]
        wrr = [0]

        def wslot():
            r = wring[wrr[0] % NW]
            wrr[0] += 1
            return r

        WK = {"f1i": (22, 2048), "f1o": (8, 2816), "f2i": (22, 2048), "f2o": (8, 2816),
              "sh": (1, 2560), "rw": (4, 3072), "ml": (4, 4096), "mg": (8, 3072), "wo": (8, 1024)}
        scr = {}
        if cfg.prepass:
            for k_, (n_, c_) in WK.items():
                t_ = nc.dram_tensor("scr_" + k_, [DEPTH, n_, 128, c_], BF16, kind="Internal").ap()
                scr[k_] = [[Reg(t_[l, i], f"scr_{k_}_{l}_{i}") for i in range(n_)] for l in range(DEPTH)]

        def k8(slot, n):
            return slot.v(slot.ap[:, 0:8 * n].rearrange("p (k n) -> p k n", k=8))

        def fill_f32(kind, slot, l, i):
            def cols(wv, dst0, dsrc, col0, n, first):
                S.dma("pool", wv[:, :, dst0:dst0 + n], dsrc[l, :, col0:col0 + n].rearrange("(k p) n -> p k n", p=128),
                      join=not first)
            if kind in ("f1i", "f2i"):
                dw = d_ffn1_in if kind == "f1i" else d_ffn2_in
                wv = k8(slot, 256)
                cols(wv, 0, dw, i * 128, 128, True)
                cols(wv, 128, dw, DFF + i * 128, 128, False)
            elif kind in ("f1o", "f2o"):
                dw = d_ffn1_out if kind == "f1o" else d_ffn2_out
                wv = slot.v(slot.ap[:, 0:22 * 128].rearrange("p (j n) -> p j n", j=22))
                S.dma("pool", wv, dw[l, :, i * 128:(i + 1) * 128].rearrange("(j p) n -> p j n", p=128))
            elif kind == "sh":
                wv = k8(slot, 320)
                cols(wv, 0, d_win, 1536, 128, True)
                cols(wv, 128, d_win, 1664, 160, False)
                if l > 0:
                    S.dma("pool", wv[:, :, 288:320], d_vd[l - 1].rearrange("(k p) n -> p k n", p=128), join=True)
            elif kind == "rw":
                wv = k8(slot, 384)
                cols(wv, 0, d_win, i * 128, 128, True)
                cols(wv, 128, d_win, 512 + i * 128, 128, False)
                cols(wv, 256, d_win, 1024 + i * 128, 128, False)
            elif kind == "ml":
                wv = k8(slot, 512)
                cols(wv, 0, d_win, 1824 + i * 128, 128, True)
                cols(wv, 128, d_win, 2336 + i * 128, 128, False)
                cols(wv, 256, d_win, 2848 + i * 128, 128, False)
                cols(wv, 384, d_win, 3360 + i * 128, 128, False)
            elif kind == "mg":
                bra = slot.v(slot.ap[:, 0:512].rearrange("p (k n) -> p k n", k=4))
                brb = slot.v(slot.ap[:, 512:1024].rearrange("p (k n) -> p k n", k=4))
                ga = slot.v(slot.ap[:, 1024:2048].rearrange("p (k n) -> p k n", k=8))
                gb = slot.v(slot.ap[:, 2048:3072].rearrange("p (k n) -> p k n", k=8))
                S.dma("pool", bra, d_bra[l, :, i * 128:(i + 1) * 128].rearrange("(k p) n -> p k n", p=128))
                S.dma("pool", brb, d_brb[l, :, i * 128:(i + 1) * 128].rearrange("(k p) n -> p k n", p=128), join=True)
                S.dma("pool", ga, d_win[l, :, 3880 + i * 128:3880 + (i + 1) * 128].rearrange("(k p) n -> p k n", p=128), join=True)
                S.dma("pool", gb, d_win[l, :, 4904 + i * 128:4904 + (i + 1) * 128].rearrange("(k p) n -> p k n", p=128), join=True)
            elif kind == "wo":
                wv = k8(slot, 128)
                S.dma("pool", wv, d_wout[l, :, i * 128:(i + 1) * 128].rearrange("(k p) n -> p k n", p=128))

        def wtile(kind, l, i):
            slot = wslot()
            if cfg.prepass:
                S.dma("sp", slot[:, 0:WK[kind][1]], scr[kind][l][i])
            else:
                fill_f32(kind, slot, l, i)
            return slot

        def prepass():
            for l in range(DEPTH):
                for kind, (n_, c_) in WK.items():
                    for i in range(n_):
                        slot = wslot()
                        fill_f32(kind, slot, l, i)
                        S.dma("sp", scr[kind][l][i], slot[:, 0:c_])

        WAw = R([128, 512], BF16, "WAw")
        WAa = R([128, 512], BF16, "WAa")
        G0w = R([128, 512], BF16, "G0w")
        G1w = R([32, 512], BF16, "G1w")
        VUw = R([32, 512], BF16, "VUw")
        memset("dve", WAw, 0.0)
        memset("dve", WAa, 0.0)
        Wg = R([128, 8, 8], BF16, "Wg")

        HID_COLS = 22 * SEG
        ar_used = [0]
        ARENA_F32 = 23056
        arena = sb([128, ARENA_F32], F32, "arena")
        mix_regs = []

        def AR(parts, cols, dt=F32, name=""):
            ncol32 = cols if dt == F32 else (cols + 1) // 2
            a = arena[0:parts, ar_used[0]:ar_used[0] + ncol32]
            ar_used[0] += ncol32
            assert ar_used[0] <= ARENA_F32, "arena overflow"
            if dt != F32:
                a = a.bitcast(BF16)
            r = Reg(a, name)
            mix_regs.append(r)
            return r

        NF = 15
        Fw = [AR(128, 512, F32, f"F{i}") for i in range(3)]
        Bw = [AR(128, 512, BF16, f"B{i}") for i in range(2)]
        shared_regs = list(mix_regs)
        hid_off = ar_used[0]
        hid_bf = arena[:, hid_off:hid_off + HID_COLS // 2].bitcast(BF16)
        hid = [[Reg(hid_bf[:, (j * NTB + tb) * 512:(j * NTB + tb + 1) * 512]) for tb in range(NTB)] for j in range(22)]
        mix_regs = []
        Fw += [AR(128, 512, F32, f"F{i}") for i in range(3, NF)]
        LT = [AR(128, 512, F32, f"LT{i}") for i in range(2)]
        Bw += [AR(128, 512, BF16, f"B{i}") for i in range(2, 8)]
        ARpA = AR(128, 1024, BF16, "ARpA")
        ARpB = AR(128, 1024, BF16, "ARpB")
        vpad = AR(64, 2048, BF16, "vpad")
        Btp = AR(64, 2048, BF16, "Btp")
        Ktp = AR(64, 2048, BF16, "Ktp")
        Nb = [AR(64, 1024, BF16, f"Nb{h_}") for h_ in range(2)]
        Nk = [AR(64, 1024, BF16, f"Nk{h_}") for h_ in range(2)]
        Qb = [[AR(64, 512, BF16, f"Qb{q_}{i}") for i in range(2)] for q_ in range(2)]
        Pb = [[AR(64, 512, BF16, f"Pb{q_}{i}") for i in range(2)] for q_ in range(2)]
        Tq = [AR(64, 512, BF16, f"Tq{i}") for i in range(2)]
        Xs = AR(64, 128, BF16, "Xs")
        Unp = AR(64, 256, BF16, "Unp")
        Sbf = AR(128, 128, BF16, "Sbf")
        SPt = AR(128, 128, F32, "SPt")
        CNbf = AR(128, 256, BF16, "CNbf")
        CNe = AR(128, 256, F32, "CNe")
        zq = AR(128, 516, F32, "zq")
        zk = AR(128, 516, F32, "zk")
        vt1 = AR(64, 2048, BF16, "vt1")
        kto = AR(64, 1024, BF16, "kto")
        sTm = AR(64, 512, BF16, "sTm")
        Gg = [Fw[9 + i][0:4, :] for i in range(4)]
        hid_regs = [r for row in hid for r in row]

        def zero_pads():
            memset("dve", ARpA, 0.0)
            memset("dve", ARpB, 0.0)
            memset("dve", vpad, 0.0)
            memset("dve", Btp, 0.0)
            memset("dve", Ktp, 0.0)
            memset("dve", Unp, 0.0)
            memset("dve", vt1, 1.0)

        def to_mixer():
            S.transfer_hazards(hid_regs, mix_regs)
            zero_pads()

        def to_ffn():
            S.transfer_hazards(mix_regs, hid_regs)

        def zero_pads_unused():
            memset("dve", ARpA, 0.0)
            memset("dve", ARpB, 0.0)
            memset("dve", vpad, 0.0)
            memset("dve", Btp, 0.0)
            memset("dve", Ktp, 0.0)
            memset("dve", Unp, 0.0)
            memset("dve", vt1, 1.0)

        def rmsnorm(l, gbase, tb, dst=None):
            ps = P.alloc()
            for c in range(8):
                sq = Bw[c % 2] if dst is None else Bw[c % 2]
                act(sq, h[c][tb], AF.Square)
                mm(ps, ones_bf, sq, c == 0, c == 7)
            rstd = Fw[2]
            act(rstd, ps, AF.Sqrt, bias=1e-6, scale=1.0 / D)
            P.release(ps)
            recip(rstd, rstd)
            for c in range(8):
                o = xn[c][tb] if dst is None else h[c][tb]
                stt("dve", o, h[c][tb], pcol(l, gbase + c), rstd, MULT, MULT)

        def ffn(l, which, gbase):
            for tb in range(NTB):
                rmsnorm(l, gbase, tb)
            for j in range(22):
                wt = wtile("f%di" % which, l, j)
                wv = k8(wt, 256)
                for tb in range(NTB):
                    pg = P.alloc()
                    pu = P.alloc()
                    for k in range(8):
                        mm(pg, wv[:, k, 0:128], xn[k][tb], k == 0, k == 7)
                    for k in range(8):
                        mm(pu, wv[:, k, 128:256], xn[k][tb], k == 0, k == 7)
                    sg = Fw[j % 2]
                    act(sg, pg, AF.Silu)
                    tt("dve", hid[j][tb], sg, pu, MULT)
                    P.release(pg)
                    P.release(pu)
            for c in range(8):
                wt = wtile("f%do" % which, l, c)
                wv = wt.v(wt.ap[:, 0:22 * 128].rearrange("p (j n) -> p j n", j=22))
                for tb in range(NTB):
                    ps = P.alloc()
                    for j in range(22):
                        mm(ps, wv[:, j, :], hid[j][tb], j == 0, j == 21)
                    stt("dve", h[c][tb], ps, 0.5, h[c][tb], MULT, ADD)
                    P.release(ps)

        lt_rr = [0]

        def lerp(ps, rows, l, ci, out):
            tmp = LT[lt_rr[0] % 2]
            lt_rr[0] += 1
            mu = pv[0:rows, l, PV_MU + ci:PV_MU + ci + 1]
            omu = pvd[0:rows, l, ci:ci + 1]
            cr = car[l][ci]
            act(tmp[0:rows, 1:512], ps[0:rows, 0:511], AF.Copy, scale=mu)
            act(tmp[0:rows, 0:1], cr[0:rows, :], AF.Copy, scale=mu)
            stt("dve", out, ps[0:rows, :], omu, tmp[0:rows, :], MULT, ADD)
            act(cr[0:rows, :], ps[0:rows, 511:512], AF.Copy)

        def proj(wv, c0, c1, tb, rows=128):
            ps = P.alloc()
            for k in range(8):
                mm(ps[0:rows, :], wv[:, k, c0:c1], xn[k][tb], k == 0, k == 7)
            return ps

        def load_cols(wv, dst0, dsrc, l, col0, n, first):
            S.dma("pool", wv[:, :, dst0:dst0 + n], dsrc[l, :, col0:col0 + n].rearrange("(k p) n -> p k n", p=128),
                  join=not first)

        def mixer(l):
            for tb in range(NTB):
                rmsnorm(l, PV_MIX, tb)
            S.dma("pool", WAw[0:64, :], d_wup[l])
            S.dma("pool", WAa[64:128, :], d_aup[l])
            S.dma("pool", G0w, d_gup[l, 0:128, :])
            S.dma("pool", G1w, d_gup[l, 128:160, :])
            if l > 0:
                S.dma("pool", VUw, d_vu[l - 1])
            wt = wtile("sh", l, 0)
            wv = k8(wt, 320)
            S.dma("pool", Wg, d_win[l, :, 3872:3880].rearrange("(k p) n -> p k n", p=128))
            for tb in range(NTB):
                if cfg.do_rw:
                    ps = proj(wv, 0, 128, tb)
                    t_ = Fw[0]
                    lerp(ps, 128, l, 12, t_)
                    P.release(ps)
                    act(lora[0][tb][0:64, :], t_[0:64, :], AF.Tanh)
                    cp("act", lora[0][tb][64:128, :], t_[64:128, :])
                    ps = proj(wv, 128, 256, tb)
                    lerp(ps, 128, l, 13, t_)
                    P.release(ps)
                    act(lora[1][tb], t_, AF.Sigmoid)
                    ps = proj(wv, 256, 288, tb, rows=32)
                    lerp(ps, 32, l, 14, t_[0:32, :])
                    P.release(ps)
                    act(lora[2][tb][0:32, :], t_[0:32, :], AF.Sigmoid)
                    if l > 0:
                        ps = proj(wv, 288, 320, tb, rows=32)
                        cp("act", lora[3][tb][0:32, :], ps[0:32, :])
                        P.release(ps)
            if cfg.do_rw:
                for hp in range(4):
                    rwkv_pair(l, hp)
            else:
                for hp in range(4):
                    for tb in range(NTB):
                        memset("dve", ya[hp][tb], 0.0)
            if cfg.do_ml:
                for hd in range(4):
                    mlstm_head(l, hd)
            else:
                for hd in range(4):
                    for tb in range(NTB):
                        memset("dve", yb[hd][tb], 0.0)
            for oc in range(8):
                wt2 = wtile("mg", l, oc)
                bra = wt2.v(wt2.ap[:, 0:512].rearrange("p (k n) -> p k n", k=4))
                brb = wt2.v(wt2.ap[:, 512:1024].rearrange("p (k n) -> p k n", k=4))
                ga = wt2.v(wt2.ap[:, 1024:2048].rearrange("p (k n) -> p k n", k=8))
                gb = wt2.v(wt2.ap[:, 2048:3072].rearrange("p (k n) -> p k n", k=8))
                for tb in range(NTB):
                    pa = P.alloc()
                    for k in range(4):
                        mm(pa, bra[:, k, :], ya[k][tb], k == 0, k == 3)
                    pb = P.alloc()
                    for k in range(4):
                        mm(pb, brb[:, k, :], yb[k][tb], k == 0, k == 3)
                    pga = P.alloc()
                    for k in range(8):
                        mm(pga, ga[:, k, :], xn[k][tb], k == 0, k == 7)
                    t1, t2 = Fw[0], Fw[1]
                    act(t1, pga, AF.Sigmoid)
                    P.release(pga)
                    pgb = P.alloc()
                    for k in range(8):
                        mm(pgb, gb[:, k, :], xn[k][tb], k == 0, k == 7)
                    act(t2, pgb, AF.Sigmoid)
                    P.release(pgb)
                    tt("dve", t1, t1, pa, MULT)
                    tt("dve", t2, t2, pb, MULT)
                    tt("dve", u[oc][tb], t1, t2, ADD)
                    P.release(pa)
                    P.release(pb)
            for oc in range(8):
                wt2 = wtile("wo", l, oc)
                wo = k8(wt2, 128)
                for tb in range(NTB):
                    ps = P.alloc()
                    for k in range(8):
                        mm(ps, wo[:, k, :], u[k][tb], k == 0, k == 7)
                    tt("dve", h[oc][tb], h[oc][tb], ps, ADD)
                    P.release(ps)

        def rwkv_pair(l, hp):
            wt = wtile("rw", l, hp)
            wv = k8(wt, 384)
            Rr, Kk, Vv, SGW, Aa, KK, T1, T2, CUM, EP, EM, EX, BON, Gt, Yt = Fw
            Rt, Kt, Bt, At, Vbf, Tb1, Tb2, _ = Bw
            ARp = (ARpA, ARpB)
            csl = slice(hp * 128, (hp + 1) * 128)
            for tb in range(NTB):
                for idx, (dst, ci) in enumerate(((Rr, hp), (Kk, 4 + hp), (Vv, 8 + hp))):
                    ps = proj(wv, idx * 128, (idx + 1) * 128, tb)
                    lerp(ps, 128, l, ci, dst)
                    P.release(ps)
                ps = P.alloc()
                mm(ps, WAw[:, csl], lora[0][tb])
                act(SGW, ps, AF.Sigmoid, bias=pcol(l, PV_W0 + hp))
                P.release(ps)
                ps = P.alloc()
                mm(ps, WAa[:, csl], lora[0][tb])
                act(Aa, ps, AF.Sigmoid, bias=pcol(l, PV_A0 + hp))
                P.release(ps)
                ps = P.alloc()
                mm(ps, G0w[:, csl], lora[1][tb], True, False)
                mm(ps, G1w[0:32, csl], lora[2][tb][0:32, :], False, True)
                cp("act", Gt, ps)
                P.release(ps)
                if l > 0:
                    ps = P.alloc()
                    mm(ps, VUw[0:32, csl], lora[3][tb][0:32, :])
                    act(T1, ps, AF.Sigmoid, bias=pcol(l, PV_VB + hp))
                    P.release(ps)
                    tt("dve", T2, vfirst[hp][tb], Vv, SUB)
                    tt("dve", T2, T2, T1, MULT)
                    tt("dve", Vv, Vv, T2, ADD)
                else:
                    cp("act", vfirst[hp][tb], Vv)
                ts("dve", KK, Kk, pcol(l, PV_KK + hp), None, MULT)
                tt("dve", Tb1, KK, KK, MULT)
                ps = P.alloc()
                mm(ps, bd_bf, Tb1)
                act(T1, ps, AF.Sqrt)
                P.release(ps)
                ts("dve", T1, T1, 1e-12, None, MAXOP)
                recip(T1, T1)
                tt("dve", KK, KK, T1, MULT)
                ts("dve", T1, Aa, pcol(l, PV_KA + hp), pvd[:, l, 15 + hp:16 + hp], MULT, ADD)
                tt("dve", Kk, Kk, T1, MULT)
                stt("dve", Tb2, Rr, pcol(l, PV_RK + hp), Kk, MULT, MULT)
                ps = P.alloc()
                mm(ps, bd_bf, Tb2)
                tt("dve", BON, ps, Vv, MULT)
                P.release(ps)
                scan(CUM, resetm, SGW)
                act(EP, CUM, AF.Exp, scale=-C0)
                act(EM, CUM, AF.Exp, scale=C0)
                tt("dve", T1, CUM, SGW, SUB)
                act(EX, T1, AF.Exp, scale=-C0)
                tt("dve", Rt, Rr, EP, MULT)
                tt("dve", Kt, Kk, EM, MULT)
                tt("dve", T1, KK, Aa, MULT)
                tt("dve", Bt, T1, EM, MULT)
                tt("dve", At, KK, EX, MULT)
                cp("act", Vbf, Vv)
                for h_ in range(2):
                    psl = slice(64 * h_, 64 * h_ + 64)
                    av = ARp[h_].v(ARp[h_].ap.rearrange("p (c two t) -> p c two t", c=8, two=2))
                    cp("act", av[psl, :, 0, :], At.v(At.ap.rearrange("p (c t) -> p c t", c=8))[psl])
                    cp("act", av[psl, :, 1, :], Rt.v(Rt.ap.rearrange("p (c t) -> p c t", c=8))[psl])
                for src, dstp in ((Vbf, vpad), (Bt, Btp), (Kt, Ktp)):
                    dv = dstp.v(dstp.ap.rearrange("p (c two n) -> p c two n", c=8, two=2))
                    for q in range(2):
                        ps = P.alloc()
                        for cc in range(4):
                            c = q * 4 + cc
                            mm(ps[0:64, cc * 128:(cc + 1) * 128], src[:, c * 64:(c + 1) * 64], ident_bf)
                        psv = ps.v(ps.ap[0:64, :].rearrange("p (c n) -> p c n", c=4))
                        cp("act", dv[:, q * 4:q * 4 + 4, 0, 0:64], psv[:, :, 0:64])
                        cp("dve", dv[:, q * 4:q * 4 + 4, 1, 64:128], psv[:, :, 64:128])
                        P.release(ps)
                NbV = [x.v(x.ap.rearrange("p (c n) -> p c n", c=8)) for x in Nb]
                NkV = [x.v(x.ap.rearrange("p (c n) -> p c n", c=8)) for x in Nk]
                ARV = [x.v(x.ap.rearrange("p (c n) -> p c n", c=8)) for x in ARp]
                idv = identrep.rearrange("p (c t) -> p c t", c=8)
                def blk(x, i8):
                    return x[:, i8 * 64:(i8 + 1) * 64]

                for q in range(2):
                    for h_ in range(2):
                        psb = P.alloc()
                        psk = P.alloc()
                        for cc in range(4):
                            c = q * 4 + cc
                            mm(psb[0:64, cc * 128:(cc + 1) * 128], Bt[:, c * 64:(c + 1) * 64], ARV[h_][:, c, :])
                            mm(psk[0:64, cc * 128:(cc + 1) * 128], Kt[:, c * 64:(c + 1) * 64], ARV[h_][:, c, :])
                        tt("dve", Nb[h_][:, q * 512:(q + 1) * 512], psb[0:64, :], mask_si, MULT)
                        tt("dve", Nk[h_][:, q * 512:(q + 1) * 512], psk[0:64, :], mask_si, MULT)
                        P.release(psb)
                        P.release(psk)
                    pst = P.alloc()
                    for h_ in range(2):
                        for cc in range(4):
                            c = q * 4 + cc
                            i8 = h_ * 4 + cc
                            mm(pst[0:64, i8 * 64:(i8 + 1) * 64], ARV[h_][:, c, 0:64], Bt[:, c * 64:(c + 1) * 64])
                    tt("dve", Qb[q][0], pst[0:64, :], mask_sl, MULT)
                    P.release(pst)
                    TtV = Tq[q].v(Tq[q].ap.rearrange("p (h c t) -> p h c t", h=2, c=4))
                    for h_ in range(2):
                        tt("dve", TtV[:, h_, :, :], idv[:, 0:4, :], NbV[h_][:, q * 4:q * 4 + 4, 0:64], SUB)
                Pm = [(lambda q_: (lambda i8: NbV[i8 // 4][:, q_ * 4 + i8 % 4, 0:64]))(q) for q in range(2)]
                Qm = [(lambda q_: (lambda i8: blk(Qb[q_][0], i8)))(q) for q in range(2)]
                for lev in range(5):
                    for q in range(2):
                        Tt = Tq[q]
                        newQ = Qb[q][(lev + 1) % 2]
                        psq = P.alloc()
                        for i8 in range(8):
                            mm(blk(psq[0:64, :], i8), Pm[q](i8), Qm[q](i8))
                        cp("act", newQ, psq[0:64, :])
                        P.release(psq)
                        if lev < 4:
                            newP = Pb[q][lev % 2]
                            psp = P.alloc()
                            for i8 in range(8):
                                mm(blk(psp[0:64, :], i8), Qm[q](i8), Pm[q](i8))
                            cp("act", newP, psp[0:64, :])
                            P.release(psp)
                        pt2 = P.alloc()
                        for i8 in range(8):
                            mm(blk(pt2[0:64, :], i8), blk(newQ, i8), blk(Tt, i8))
                        tt("dve", Tt, Tt, pt2[0:64, :], ADD)
                        P.release(pt2)
                        if lev < 4:
                            Pm[q] = (lambda np_: (lambda i8: blk(np_, i8)))(newP)
                        Qm[q] = (lambda nq_: (lambda i8: blk(nq_, i8)))(newQ)
                Sreg = Sst[l][hp]
                cp("act", Sbf, Sreg)
                vpv = vpad.v(vpad.ap.rearrange("p (c two n) -> p c two n", c=8, two=2))
                bpv = Btp.v(Btp.ap.rearrange("p (c two n) -> p c two n", c=8, two=2))
                kpv = Ktp.v(Ktp.ap.rearrange("p (c two n) -> p c two n", c=8, two=2))
                unv = Unp.v(Unp.ap.rearrange("p (two n) -> p two n", two=2))
                un4 = Unp.v(Unp.ap.rearrange("p (b n) -> p b n", b=4))
                psY = P.alloc()
                for c in range(8):
                    q, cc = c // 4, c % 4
                    ck = slice(c * 64, (c + 1) * 64)
                    psX = P.alloc()
                    mm(psX[0:64, 0:128], At[:, ck], Sbf, True, False)
                    mm(psX[0:64, 0:128], NkV[0][:, c, 0:64], vpv[:, c, 0, :], False, False)
                    mm(psX[0:64, 0:128], NkV[1][:, c, 0:64], vpv[:, c, 1, :], False, True)
                    cp("act", Xs, psX[0:64, 0:128])
                    P.release(psX)
                    psU = P.alloc()
                    mm(psU[0:64, 0:64], blk(Tq[q], cc), Xs[:, 0:64])
                    mm(psU[0:64, 64:128], blk(Tq[q], 4 + cc), Xs[:, 64:128])
                    ts("dve", un4[:, 0:4:3, :], psU.v(psU.ap[0:64, 0:128].rearrange("p (two n) -> p two n", two=2)),
                       -1.0, None, MULT)
                    P.release(psU)
                    mm(psY[:, ck], Sbf, Rt[:, ck], True, False)
                    mm(psY[:, ck], unv[:, 0, :], NbV[0][:, c, 64:128], False, False)
                    mm(psY[:, ck], unv[:, 1, :], NbV[1][:, c, 64:128], False, False)
                    mm(psY[:, ck], vpv[:, c, 0, :], NkV[0][:, c, 64:128], False, False)
                    mm(psY[:, ck], vpv[:, c, 1, :], NkV[1][:, c, 64:128], False, True)
                    psS = P.alloc()
                    mm(psS[:, 0:128], bpv[:, c, 0, :], unv[:, 0, :], True, False)
                    mm(psS[:, 0:128], bpv[:, c, 1, :], unv[:, 1, :], False, False)
                    mm(psS[:, 0:128], kpv[:, c, 0, :], vpv[:, c, 0, :], False, False)
                    mm(psS[:, 0:128], kpv[:, c, 1, :], vpv[:, c, 1, :], False, True)
                    pL = EP[:, c * 64 + 63:c * 64 + 64]
                    ts("dve", SPt, Sreg, pL, None, MULT)
                    stt("dve", Sbf, psS[:, 0:128], pL, SPt, MULT, ADD)
                    stt("dve", Sreg, psS[:, 0:128], pL, SPt, MULT, ADD)
                    P.release(psS)
                cp("act", Yt, psY)
                P.release(psY)
                cp("act", Tb1, Yt)
                tt("dve", Tb2, Yt, Yt, MULT)
                ps1 = P.alloc()
                mm(ps1, bd_bf, Tb1)
                ps2 = P.alloc()
                mm(ps2, bd_bf, Tb2)
                ts("dve", T1, ps1, 1.0 / 64, None, MULT)
                tt("dve", T2, T1, T1, MULT)
                stt("dve", T2, ps2, 1.0 / 64, T2, MULT, SUB)
                P.release(ps1)
                P.release(ps2)
                act(T2, T2, AF.Sqrt, bias=64e-5, scale=1.0)
                recip(T2, T2)
                tt("dve", Yt, Yt, T1, SUB)
                tt("dve", Yt, Yt, T2, MULT)
                ts("dve", Yt, Yt, pcol(l, PV_GNW + hp), pcol(l, PV_GNB + hp), MULT, ADD)
                tt("dve", Yt, Yt, BON, ADD)
                tt("dve", ya[hp][tb], Yt, Gt, MULT)

        def mlstm_head(l, hd):
            LI, LF, BC, G1 = Gg
            GT = LF
            wt = wtile("ml", l, hd)
            wv = k8(wt, 512)
            Qc, Kc, SO, ALPHA, BETA, HH, T1, T2, MEAN = Fw[0:9]
            QT, KT, Vbf, Tb1, Tb2 = Bw[0:5]
            CN = CNst[l][hd]
            for tb in range(NTB):
                if hd == 0 or NTB > 1:
                    ps = proj(Wg, 0, 4, tb, rows=4)
                    ts("dve", LI, ps[0:4, :], pv[0:4, l, PV_IB:PV_IB + 1], None, ADD)
                    P.release(ps)
                    ps = proj(Wg, 4, 8, tb, rows=4)
                    act(GT, ps[0:4, :], AF.Exp, bias=pvd[0:4, l, 19:20], scale=-1.0)
                    P.release(ps)
                    act(GT, GT, AF.Ln, bias=1.0)
                    ts("dve", LF, GT, -1.0, None, MULT)
                    scan(BC, resetm[0:4, :], LF)
                    tt("dve", G1, LI, BC, SUB)
                for (zz, dst, idx) in ((zq, Qc, 0), (zk, Kc, 1)):
                    ci = idx * 4 + hd
                    ps = proj(wv, idx * 128, (idx + 1) * 128, tb)
                    cp("act", zz[:, 3:515], ps)
                    P.release(ps)
                    cp("act", zz[:, 0:3], cqk[l][ci])
                    ts("dve", dst, zz[:, 0:512], pcol(l, PV_CW + ci), pcol(l, PV_CB + ci), MULT, ADD)
                    for j in range(1, 4):
                        stt("dve", dst, zz[:, j:j + 512], pcol(l, PV_CW + 8 * j + ci), dst, MULT, ADD)
                    cp("act", cqk[l][ci], zz[:, 512:515])
                    act(dst, dst, AF.Silu)
                ps = proj(wv, 256, 384, tb)
                cp("act", Vbf, ps)
                P.release(ps)
                ps = proj(wv, 384, 512, tb)
                act(SO, ps, AF.Sigmoid)
                P.release(ps)
                ps = P.alloc()
                mm(ps, sel4[:, hd * 128:(hd + 1) * 128], BC)
                act(ALPHA, ps, AF.Exp)
                P.release(ps)
                ps = P.alloc()
                mm(ps, sel4[:, hd * 128:(hd + 1) * 128], G1)
                act(BETA, ps, AF.Exp)
                P.release(ps)
                stt("dve", QT, Qc, 128.0 ** -0.5, ALPHA, MULT, MULT)
                tt("dve", KT, Kc, BETA, MULT)
                v1 = vt1.v(vt1.ap.rearrange("p (c n) -> p c n", c=8))
                ktv = kto.v(kto.ap.rearrange("p (c n) -> p c n", c=8))
                for q in range(2):
                    ps = P.alloc()
                    for cc in range(4):
                        c = q * 4 + cc
                        mm(ps[0:64, cc * 128:(cc + 1) * 128], Vbf[:, c * 64:(c + 1) * 64], ident_bf)
                    cp("act", v1[:, q * 4:q * 4 + 4, 0:128], ps.v(ps.ap[0:64, :].rearrange("p (c n) -> p c n", c=4)))
                    P.release(ps)
                    ps = P.alloc()
                    for cc in range(4):
                        c = q * 4 + cc
                        mm(ps[0:64, cc * 128:(cc + 1) * 128], KT[:, c * 64:(c + 1) * 64], ident_bf)
                    cp("act", kto[:, q * 512:(q + 1) * 512], ps[0:64, :])
                    P.release(ps)
                ps = P.alloc()
                for c in range(8):
                    ck = slice(c * 64, (c + 1) * 64)
                    mm(ps[0:64, ck], KT[:, ck], QT[:, ck])
                tt("dve", sTm, ps[0:64, :], mask_incl, MULT)
                P.release(ps)
                cp("act", CNbf, CN)
                psN = P.alloc()
                psD = P.alloc()
                for c in range(8):
                    ck = slice(c * 64, (c + 1) * 64)
                    mm(psN[:, ck], v1[:, c, 0:128], sTm[:, ck], True, False)
                    mm(psN[:, ck], CNbf[:, 0:128], QT[:, ck], False, True)
                    mm(psD[:, ck], ones_bf[0:64, :], sTm[:, ck], True, False)
                    mm(psD[:, ck], CNbf[:, 128:256], QT[:, ck], False, True)
                    psC = P.alloc()
                    mm(psC[:, 0:256], ktv[:, c, :], v1[:, c, :])
                    ebL = ALPHA[:, c * 64 + 63:c * 64 + 64]
                    ts("dve", CNe, CN, ebL, None, MULT)
                    stt("dve", CNbf, psC[:, 0:256], ebL, CNe, MULT, ADD)
                    stt("dve", CN, psC[:, 0:256], ebL, CNe, MULT, ADD)
                    P.release(psC)
                act(T1, psD, AF.Abs)
                P.release(psD)
                ts("dve", T1, T1, 1.0, None, MAXOP)
                recip(T1, T1)
                tt("dve", HH, psN, T1, MULT)
                P.release(psN)
                cp("act", Tb1, HH)
                tt("dve", Tb2, HH, HH, MULT)
                ps1 = P.alloc()
                mm(ps1, ones_bf, Tb1)
                ps2 = P.alloc()
                mm(ps2, ones_bf, Tb2)
                ts("dve", MEAN, ps1, 1.0 / 128, None, MULT)
                tt("dve", T2, MEAN, MEAN, MULT)
                stt("dve", T2, ps2, 1.0 / 128, T2, MULT, SUB)
                P.release(ps1)
                P.release(ps2)
                act(T2, T2, AF.Sqrt, bias=1e-5, scale=1.0)
                recip(T2, T2)
                tt("dve", HH, HH, MEAN, SUB)
                tt("dve", HH, HH, T2, MULT)
                stt("dve", yb[hd][tb], HH, pcol(l, PV_NW + hd), SO, MULT, MULT)

        out_toks = []
        if cfg.prepass:
            prepass()
        for s in range(NS):
            for rg in state_all:
                memset("dve", rg, 0.0)
            for g in range(NSEG):
                t0 = g * SEG
                for c in range(8):
                    for tb in range(NTB):
                        S.dma("sp", h[c][tb], xT[s, c * 128:(c + 1) * 128, t0 + tb * 512:t0 + (tb + 1) * 512])
                for l in range(DEPTH):
                    if cfg.do_ffn:
                        to_ffn()
                        ffn(l, 1, PV_FFN1)
                    if cfg.do_mix:
                        to_mixer()
                        mixer(l)
                    if cfg.do_ffn:
                        to_ffn()
                        ffn(l, 2, PV_FFN2)
                for tb in range(NTB):
                    rmsnorm(0, PV_FIN, tb, dst="h")
                    for c in range(8):
                        out_toks.append(S.dma("sp", outT[s, c * 128:(c + 1) * 128, t0 + tb * 512:t0 + (tb + 1) * 512],
                                              h[c][tb]))
        S.wait_all_at_end("sp", [("dma", k, 16 * S.dma_cnt[k]) for k in range(S.n_dma_sems) if S.dma_cnt[k]])
        S.emit(st)
    return nc


_CACHE = {}


def run_cores(inp, cfg, n_cores):
    depth = cfg.DEPTH
    key = (cfg.NS, cfg.T, cfg.DEPTH, cfg.NTB, cfg.do_ffn, cfg.do_rw, cfg.do_ml, cfg.do_mix, cfg.prepass)
    if key not in _CACHE:
        _CACHE[key] = build_program(cfg)
    nc = _CACHE[key]
    pv = pack_params(inp, depth)
    consts = make_consts()
    def f(k):
        a = np.ascontiguousarray(inp[k], dtype=np.float32)
        if a.shape[0] == 0:
            a = np.zeros((1,) + a.shape[1:], np.float32)
        return a
    shared = {
        "ffn1_w_in": f("ffn1_w_in"), "ffn1_w_out": f("ffn1_w_out"), "ffn2_w_in": f("ffn2_w_in"),
        "ffn2_w_out": f("ffn2_w_out"), "w_in": f("w_in"), "rw_w_up": f("rw_w_up"), "rw_a_up": f("rw_a_up"),
        "rw_g_up": f("rw_g_up"), "vres_down": f("vres_down"), "vres_up": f("vres_up"), "br_a": f("br_a"),
        "br_b": f("br_b"), "w_out": f("w_out"), "pvec": pv, "consts": consts,
    }
    x = np.asarray(inp["x"], np.float32)
    in_maps = []
    for c in range(n_cores):
        xs = x[c * cfg.NS:(c + 1) * cfg.NS]
        m = dict(shared)
        m["xT"] = np.ascontiguousarray(xs.transpose(0, 2, 1))
        in_maps.append(m)
    res = run_bass_kernel_spmd(nc, in_maps, core_ids=list(range(n_cores)))
    outs = [np.asarray(r["outT"]).transpose(0, 2, 1) for r in res.results]
    return np.ascontiguousarray(np.concatenate(outs, axis=0), dtype=np.float32)


def kernel(**inputs):
    cfg = Cfg(NS=4, T=2048, DEPTH=4, NTB=1)
    return run_cores(inputs, cfg, 8)
```

```python
import numpy as np
from contextlib import ExitStack
import concourse.bass as bass
import concourse.mybir as mybir
from concourse.bass_utils import run_bass_kernel_spmd

F32 = mybir.dt.float32
BF16 = mybir.dt.bfloat16
AF = mybir.ActivationFunctionType
ALU = mybir.AluOpType
MULT, ADD, SUB, MAXOP = ALU.mult, ALU.add, ALU.subtract, ALU.max
EPOCH = 16000
ENGS = ("pe", "act", "dve", "pool", "sp")

D = 1024
DFF = 2816
NIN = 5928
C0 = float(np.exp(-0.5))


class Reg:
    __slots__ = ("ap", "w", "rs", "name", "track", "psum")

    def __init__(self, ap, name="", track=True, psum=False):
        self.ap = ap
        self.w = []
        self.rs = {}
        self.name = name
        self.track = track
        self.psum = psum

    def __getitem__(self, idx):
        return View(self, self.ap[idx])

    @property
    def reg(self):
        return self

    def v(self, ap):
        return View(self, ap)


class View:
    __slots__ = ("reg", "ap")

    def __init__(self, reg, ap):
        self.reg = reg
        self.ap = ap

    def __getitem__(self, idx):
        return View(self.reg, self.ap[idx])

    def rearrange(self, *a, **k):
        return View(self.reg, self.ap.rearrange(*a, **k))

    def bitcast(self, dt):
        return View(self.reg, self.ap.bitcast(dt))


def tok_key(tok):
    return tok[0] if tok[0] in ENGS else ("d", tok[1])


class Sched:
    def __init__(self, nc, n_dma_sems=32, same_engine_sync=True):
        self.nc = nc
        self.ops = {e: [] for e in ENGS}
        self.count = {e: 0 for e in ENGS}
        self.waited = {e: {} for e in ENGS}
        self.n_dma_sems = n_dma_sems
        self.dma_cnt = [0] * n_dma_sems
        self.dma_rr = 0
        self.same_engine_sync = same_engine_sync
        self.needed = {e: set() for e in ENGS}
        self.final_waits = []

    def _need(self, eng, tok, waits):
        if tok is None:
            return
        key = tok_key(tok)
        val = tok[-1]
        if key == eng and (eng == "pe" or not self.same_engine_sync):
            return
        if self.waited[eng].get(key, -1) >= val:
            return
        if key in waits and waits[key][-1] >= val:
            return
        waits[key] = tok

    def _deps(self, eng, reads, writes, waits=None):
        waits = {} if waits is None else waits
        for r in reads:
            if r.reg.track:
                for t in r.reg.w:
                    self._need(eng, t, waits)
                if r.reg.psum:
                    for k_, t in r.reg.rs.items():
                        if k_ != eng:
                            self._need(eng, t, waits)
        for w in writes:
            if w.reg.track:
                for t in w.reg.w:
                    self._need(eng, t, waits)
                for t in w.reg.rs.values():
                    self._need(eng, t, waits)
        for key, tok in waits.items():
            self.waited[eng][key] = tok[-1]
            if tok[0] in ENGS:
                self.needed[tok[0]].add(tok[1])
        return list(waits.values())

    def _commit(self, tok, reads, writes, append_w=False):
        for r in reads:
            if r.reg.track:
                r.reg.rs[tok_key(tok)] = tok
        for w in writes:
            if w.reg.track:
                if append_w:
                    w.reg.w = w.reg.w + [tok]
                else:
                    w.reg.w = [tok]
                w.reg.rs = {}

    def op(self, eng, fn, reads=(), writes=()):
        waits = self._deps(eng, reads, writes)
        idx = self.count[eng]
        self.count[eng] += 1
        tok = (eng, idx)
        self.ops[eng].append((waits, fn, tok, "c"))
        self._commit(tok, reads, writes)
        return tok

    def dma(self, eng, out, in_, join=False):
        k = self.dma_rr
        self.dma_rr = (self.dma_rr + 1) % self.n_dma_sems
        waits = {}
        if self.dma_cnt[k]:
            self._need(eng, ("dma", k, 16 * self.dma_cnt[k]), waits)
        reads, writes = [in_], [out]
        if join:
            saved = out.reg.w
            out.reg.w = []
            allw = self._deps(eng, reads, writes, waits)
            out.reg.w = saved
        else:
            allw = self._deps(eng, reads, writes, waits)
        self.dma_cnt[k] += 1
        tok = ("dma", k, 16 * self.dma_cnt[k])
        oap, iap = out.ap, in_.ap
        self.count[eng] += 1
        self.ops[eng].append((allw, lambda e: e.dma_start(out=oap, in_=iap), tok, "d"))
        self._commit(tok, reads, writes, append_w=join)
        return tok

    def transfer_hazards(self, src_regs, dst_regs):
        merged = {}
        for r in src_regs:
            for t in list(r.rs.values()) + list(r.w):
                key = tok_key(t)
                if key not in merged or merged[key][-1] < t[-1]:
                    merged[key] = t
        for d in dst_regs:
            for key, t in merged.items():
                if key not in d.rs or d.rs[key][-1] < t[-1]:
                    d.rs[key] = t

    def wait_all_at_end(self, eng, toks):
        self.final_waits.append((eng, list(toks)))

    def emit(self, st):
        nc = self.nc
        rank, sems = {}, {}
        for e in ENGS:
            ms = sorted(self.needed[e])
            rank[e] = {i: r + 1 for r, i in enumerate(ms)}
            n_ep = max(1, (len(ms) + EPOCH - 1) // EPOCH)
            sems[e] = [st.enter_context(nc.semaphore(f"s_{e}_{k}")) for k in range(n_ep)]
        dsems = [st.enter_context(nc.semaphore(f"s_dma_{k}")) for k in range(self.n_dma_sems)]

        def resolve(tok):
            if tok[0] == "dma":
                return dsems[tok[1]], tok[2]
            e, i = tok
            r = rank[e][i]
            return sems[e][(r - 1) // EPOCH], (r - 1) % EPOCH + 1

        block = st.enter_context(nc.Block())

        def make_body(e):
            def body(eng):
                for waits, fn, tok, kind in self.ops[e]:
                    ws = [resolve(w) for w in waits]
                    for s, v in ws[1:]:
                        eng.wait_ge(s, v)
                    ins = fn(eng)
                    if ws:
                        ins = ins._wait_ge(ws[0][0], ws[0][1])
                    if kind == "d":
                        ins.then_inc(dsems[tok[1]], 16)
                    elif tok[1] in rank[e]:
                        s, v = resolve(tok)
                        ins.then_inc(s, 1)
                for (fe, toks) in self.final_waits:
                    if fe == e:
                        for t in toks:
                            s, v = resolve(t)
                            eng.wait_ge(s, v)
            return body

        block.tensor(make_body("pe"))
        block.scalar(make_body("act"))
        block.vector(make_body("dve"))
        block.gpsimd(make_body("pool"))
        block.sync(make_body("sp"))


class PsumPool:
    def __init__(self, regs):
        self.free = list(regs)

    def alloc(self):
        assert self.free, "PSUM pool exhausted"
        return self.free.pop(0)

    def release(self, r):
        self.free.append(r)


PV_FFN1, PV_MIX, PV_FFN2, PV_FIN = 0, 8, 16, 24
PV_MU = 32
PV_W0, PV_A0, PV_KK, PV_KA, PV_RK, PV_GNW, PV_GNB, PV_VB = 47, 51, 55, 59, 63, 67, 71, 75
PV_CW, PV_CB, PV_IB, PV_FB, PV_NW = 79, 111, 119, 120, 121
NPV = 128


def _cols(vec, n):
    out = np.zeros((128, n), np.float32)
    v = np.asarray(vec, np.float32).reshape(-1)
    full = len(v) // 128
    if full:
        out[:, :full] = v[:full * 128].reshape(full, 128).T
    rem = len(v) - full * 128
    if rem:
        out[:rem, full] = v[full * 128:]
    return out


def pack_params(inp, depth):
    pv = np.zeros((128, depth, NPV), np.float32)
    for l in range(depth):
        p = pv[:, l]
        p[:, PV_FFN1:PV_FFN1 + 8] = _cols(inp["ffn1_norm"][l], 8)
        p[:, PV_MIX:PV_MIX + 8] = _cols(inp["mix_norm"][l], 8)
        p[:, PV_FFN2:PV_FFN2 + 8] = _cols(inp["ffn2_norm"][l], 8)
        p[:, PV_FIN:PV_FIN + 8] = _cols(inp["final_norm"], 8)
        mu = inp["shift_mu"][l]
        p[:, PV_MU:PV_MU + 12] = _cols(mu[0:1536], 12)
        p[:, PV_MU + 12:PV_MU + 13] = _cols(mu[1536:1664], 1)
        p[:, PV_MU + 13:PV_MU + 14] = _cols(mu[1664:1792], 1)
        p[:, PV_MU + 14:PV_MU + 15] = _cols(mu[1792:1824], 1)
        p[:, PV_W0:PV_W0 + 4] = _cols(inp["rw_w0"][l], 4)
        p[:, PV_A0:PV_A0 + 4] = _cols(inp["rw_a0"][l], 4)
        p[:, PV_KK:PV_KK + 4] = _cols(inp["rw_k_k"][l], 4)
        p[:, PV_KA:PV_KA + 4] = _cols(inp["rw_k_a"][l], 4)
        p[:, PV_RK:PV_RK + 4] = _cols(inp["rw_r_k"][l].reshape(-1), 4)
        p[:, PV_GNW:PV_GNW + 4] = _cols(inp["rw_gn_w"][l], 4)
        p[:, PV_GNB:PV_GNB + 4] = _cols(inp["rw_gn_b"][l], 4)
        if l > 0:
            p[:, PV_VB:PV_VB + 4] = _cols(inp["vres_bias"][l - 1], 4)
        for j in range(4):
            p[:, PV_CW + 8 * j:PV_CW + 8 * j + 8] = _cols(inp["ml_conv_w"][l][j], 8)
        p[:, PV_CB:PV_CB + 8] = _cols(inp["ml_conv_b"][l], 8)
        p[0:4, PV_IB] = inp["ml_i_bias"][l]
        p[0:4, PV_FB] = inp["ml_f_bias"][l]
        p[:, PV_NW:PV_NW + 4] = _cols(inp["ml_norm_w"][l], 4)
    return pv


def make_consts():
    c = np.zeros((128, 7, 512), np.float32)
    c[:, 0, 0:128] = np.eye(128)
    bd = np.zeros((128, 128), np.float32)
    bd[0:64, 0:64] = 1.0
    bd[64:128, 64:128] = 1.0
    c[:, 0, 128:256] = bd
    s = np.arange(64)[:, None]
    t = np.arange(64)[None, :]
    strict = (s < t).astype(np.float32)
    incl = (s <= t).astype(np.float32)
    lower = (s > t).astype(np.float32)
    c[0:64, 1] = np.tile(np.concatenate([strict, incl], axis=1), (1, 4))
    c[0:64, 2] = np.tile(lower, (1, 8))
    c[0:64, 3] = np.tile(incl, (1, 8))
    c[0:64, 4] = np.tile(np.eye(64, dtype=np.float32), (1, 8))
    r = np.ones((128, 512), np.float32)
    r[:, ::64] = 0.0
    c[:, 5] = r
    for h in range(4):
        c[h, 6, h * 128:(h + 1) * 128] = 1.0
    return c


class Cfg:
    def __init__(self, NS=4, T=2048, DEPTH=4, NTB=1, do_ffn=True, do_rw=True, do_ml=True, do_mix=True,
                 prepass=True):
        self.NS, self.T, self.DEPTH, self.NTB = NS, T, DEPTH, NTB
        self.prepass = prepass
        self.do_ffn, self.do_rw, self.do_ml, self.do_mix = do_ffn, do_rw, do_ml, do_mix


def build_program(cfg):
    NS, T, DEPTH, NTB = cfg.NS, cfg.T, cfg.DEPTH, cfg.NTB
    SEG = NTB * 512
    NSEG = T // SEG
    nc = bass.Bass("TRN2", target_bir_lowering=False)

    def din(name, shape):
        return Reg(nc.dram_tensor(name, list(shape), F32, kind="ExternalInput").ap(), name, track=False)

    xT = din("xT", [NS, D, T])
    d_ffn1_in = din("ffn1_w_in", [DEPTH, D, 2 * DFF])
    d_ffn1_out = din("ffn1_w_out", [DEPTH, DFF, D])
    d_ffn2_in = din("ffn2_w_in", [DEPTH, D, 2 * DFF])
    d_ffn2_out = din("ffn2_w_out", [DEPTH, DFF, D])
    d_win = din("w_in", [DEPTH, D, NIN])
    d_wup = din("rw_w_up", [DEPTH, 64, 512])
    d_aup = din("rw_a_up", [DEPTH, 64, 512])
    d_gup = din("rw_g_up", [DEPTH, 160, 512])
    d_vd = din("vres_down", [max(DEPTH - 1, 1), D, 32])
    d_vu = din("vres_up", [max(DEPTH - 1, 1), 32, 512])
    d_bra = din("br_a", [DEPTH, 512, D])
    d_brb = din("br_b", [DEPTH, 512, D])
    d_wout = din("w_out", [DEPTH, D, D])
    d_pv = din("pvec", [128, DEPTH, NPV])
    d_cst = din("consts", [128, 7, 512])
    outT = Reg(nc.dram_tensor("outT", [NS, D, T], F32, kind="ExternalOutput").ap(), "outT", track=False)

    S = Sched(nc)
    st = ExitStack()
    with st:
        cnt = [0]

        def sb(shape, dt=F32, name=None):
            cnt[0] += 1
            nm = name or f"t{cnt[0]}"
            return st.enter_context(nc.sbuf_tensor(nm, list(shape), dt))[:]

        def R(shape, dt=F32, name=None):
            return Reg(sb(shape, dt, name), name or "")

        def rd(x, lst):
            if hasattr(x, "reg"):
                lst.append(x)
                return x.ap
            return x

        def mm(out, lhsT, rhs, start=True, stop=True):
            o, l, r = out.ap, lhsT.ap, rhs.ap
            S.op("pe", lambda e: e.matmul(o, l, r, start=start, stop=stop), [lhsT, rhs], [out])

        def act(out, in_, func, bias=None, scale=None):
            reads = [in_]
            kw = {}
            if bias is not None:
                kw["bias"] = rd(bias, reads)
            if scale is not None:
                kw["scale"] = rd(scale, reads)
            o, i = out.ap, in_.ap
            S.op("act", lambda e: e.activation(o, i, func, **kw), reads, [out])

        def ts(eng, out, in0, s1, s2=None, op0=MULT, op1=None):
            reads = [in0]
            a1 = rd(s1, reads)
            a2 = rd(s2, reads) if s2 is not None else None
            o, i = out.ap, in0.ap
            if op1 is None:
                S.op(eng, lambda e: e.tensor_scalar(o, i, a1, None, op0), reads, [out])
            else:
                S.op(eng, lambda e: e.tensor_scalar(o, i, a1, a2, op0, op1), reads, [out])

        def tt(eng, out, in0, in1, op):
            o, a, b = out.ap, in0.ap, in1.ap
            S.op(eng, lambda e: e.tensor_tensor(o, a, b, op), [in0, in1], [out])

        def stt(eng, out, in0, scalar, in1, op0, op1):
            reads = [in0, in1]
            sc = rd(scalar, reads)
            o, a, b = out.ap, in0.ap, in1.ap
            S.op(eng, lambda e: e.scalar_tensor_tensor(o, a, sc, b, op0, op1), reads, [out])

        def cp(eng, out, in_):
            o, i = out.ap, in_.ap
            if eng == "act":
                S.op("act", lambda e: e.activation(o, i, AF.Copy), [in_], [out])
            else:
                S.op(eng, lambda e: e.tensor_copy(o, i), [in_], [out])

        def memset(eng, out, val):
            o = out.ap
            S.op(eng, lambda e: e.memset(o, val), [], [out])

        def recip(out, in_):
            o, i = out.ap, in_.ap
            S.op("dve", lambda e: e.reciprocal(o, i), [in_], [out])

        def scan(out, d0, d1):
            o, a, b = out.ap, d0.ap, d1.ap
            S.op("dve", lambda e: e.tensor_tensor_scan(o, a, b, 0.0, MULT, ADD), [d0, d1], [out])

        banks = [Reg(st.enter_context(nc.psum_tensor(f"ps{i}", [128, 512], F32))[:], f"ps{i}", psum=True) for i in range(8)]
        P = PsumPool(banks)

        cstf = R([128, 2, 512], F32, "cstf")
        S.dma("sp", cstf, d_cst[:, 5:7, :])
        cstb = R([64, 4, 512], BF16, "cstb")
        S.dma("pool", cstb, d_cst[0:64, 1:5, :])
        ident_bf = R([128, 128], BF16, "ident")
        bd_bf = R([128, 128], BF16, "bd")
        ones_bf = R([128, 128], BF16, "ones")
        S.dma("pool", ident_bf, d_cst[:, 0, 0:128])
        S.dma("pool", bd_bf, d_cst[:, 0, 128:256])
        memset("dve", ones_bf, 1.0)
        mask_si = cstb[:, 0, :]
        mask_sl = cstb[:, 1, :]
        mask_incl = cstb[:, 2, :]
        identrep = cstb[:, 3, :]
        resetm = cstf[:, 0, :]
        sel4 = cstf[0:4, 1, :]

        pv = R([128, DEPTH, NPV], F32, "pv")
        S.dma("sp", pv, d_pv)
        pvd = R([128, DEPTH, 24], F32, "pvd")
        for l in range(DEPTH):
            ts("dve", pvd[:, l, 0:15], pv[:, l, PV_MU:PV_MU + 15], -1.0, 1.0, MULT, ADD)
            ts("dve", pvd[:, l, 15:19], pv[:, l, PV_KA:PV_KA + 4], -1.0, 1.0, MULT, ADD)
            ts("dve", pvd[:, l, 19:20], pv[:, l, PV_FB:PV_FB + 1], -1.0, None, MULT)

        def pcol(l, c):
            return pv[:, l, c:c + 1]

        h_t = sb([128, 8, SEG], F32, "h")
        h = [[Reg(h_t[:, c, tb * 512:(tb + 1) * 512]) for tb in range(NTB)] for c in range(8)]
        xn_t = sb([128, 8, SEG], BF16, "xn")
        xn = [[Reg(xn_t[:, c, tb * 512:(tb + 1) * 512]) for tb in range(NTB)] for c in range(8)]
        ya_t = sb([128, 4, SEG], BF16, "ya")
        ya = [[Reg(ya_t[:, c, tb * 512:(tb + 1) * 512]) for tb in range(NTB)] for c in range(4)]
        yb_t = sb([128, 4, SEG], BF16, "yb")
        yb = [[Reg(yb_t[:, c, tb * 512:(tb + 1) * 512]) for tb in range(NTB)] for c in range(4)]
        u_t = sb([128, 8, SEG], BF16, "u")
        u = [[Reg(u_t[:, c, tb * 512:(tb + 1) * 512]) for tb in range(NTB)] for c in range(8)]
        vf_t = sb([128, 4, SEG], BF16, "vfirst")
        vfirst = [[Reg(vf_t[:, c, tb * 512:(tb + 1) * 512]) for tb in range(NTB)] for c in range(4)]
        lw_t = sb([128, 4, SEG], BF16, "lora")
        lora = [[Reg(lw_t[:, c, tb * 512:(tb + 1) * 512]) for tb in range(NTB)] for c in range(4)]

        S_t = sb([128, DEPTH, 4, 128], F32, "Sst")
        Sst = [[Reg(S_t[:, l, hp, :]) for hp in range(4)] for l in range(DEPTH)]
        CN_t = sb([128, DEPTH, 4, 256], F32, "CNst")
        CNst = [[Reg(CN_t[:, l, hd, :]) for hd in range(4)] for l in range(DEPTH)]
        car_t = sb([128, DEPTH, 16], F32, "car")
        car = [[Reg(car_t[:, l, ci:ci + 1]) for ci in range(15)] for l in range(DEPTH)]
        cqk_t = sb([128, DEPTH, 8, 3], F32, "cqk")
        cqk = [[Reg(cqk_t[:, l, c, :]) for c in range(8)] for l in range(DEPTH)]
        state_all = Reg(S_t, "Sall"), Reg(CN_t, "CNall"), Reg(car_t, "carall"), Reg(cqk_t, "cqkall")

        NW = 3
        WSZ = 4096
        wring = [R([128, WSZ], BF16, f"wslot{i}") for i in range(NW)]
        wrr = [0]

        def wslot():
            r = wring[wrr[0] % NW]
            wrr[0] += 1
            return r

        WK = {"f1i": (22, 2048), "f1o": (8, 2816), "f2i": (22, 2048), "f2o": (8, 2816),
              "sh": (1, 2560), "rw": (4, 3072), "ml": (4, 4096), "mg": (8, 3072), "wo": (8, 1024)}
        scr = {}
        if cfg.prepass:
            for k_, (n_, c_) in WK.items():
                t_ = nc.dram_tensor("scr_" + k_, [DEPTH, n_, 128, c_], BF16, kind="Internal").ap()
                scr[k_] = [[Reg(t_[l, i], f"scr_{k_}_{l}_{i}") for i in range(n_)] for l in range(DEPTH)]

        def k8(slot, n):
            return slot.v(slot.ap[:, 0:8 * n].rearrange("p (k n) -> p k n", k=8))

        def fill_f32(kind, slot, l, i):
            def cols(wv, dst0, dsrc, col0, n, first):
                S.dma("pool", wv[:, :, dst0:dst0 + n], dsrc[l, :, col0:col0 + n].rearrange("(k p) n -> p k n", p=128),
                      join=not first)
            if kind in ("f1i", "f2i"):
                dw = d_ffn1_in if kind == "f1i" else d_ffn2_in
                wv = k8(slot, 256)
                cols(wv, 0, dw, i * 128, 128, True)
                cols(wv, 128, dw, DFF + i * 128, 128, False)
            elif kind in ("f1o", "f2o"):
                dw = d_ffn1_out if kind == "f1o" else d_ffn2_out
                wv = slot.v(slot.ap[:, 0:22 * 128].rearrange("p (j n) -> p j n", j=22))
                S.dma("pool", wv, dw[l, :, i * 128:(i + 1) * 128].rearrange("(j p) n -> p j n", p=128))
            elif kind == "sh":
                wv = k8(slot, 320)
                cols(wv, 0, d_win, 1536, 128, True)
                cols(wv, 128, d_win, 1664, 160, False)
                if l > 0:
                    S.dma("pool", wv[:, :, 288:320], d_vd[l - 1].rearrange("(k p) n -> p k n", p=128), join=True)
            elif kind == "rw":
                wv = k8(slot, 384)
                cols(wv, 0, d_win, i * 128, 128, True)
                cols(wv, 128, d_win, 512 + i * 128, 128, False)
                cols(wv, 256, d_win, 1024 + i * 128, 128, False)
            elif kind == "ml":
                wv = k8(slot, 512)
                cols(wv, 0, d_win, 1824 + i * 128, 128, True)
                cols(wv, 128, d_win, 2336 + i * 128, 128, False)
                cols(wv, 256, d_win, 2848 + i * 128, 128, False)
                cols(wv, 384, d_win, 3360 + i * 128, 128, False)
            elif kind == "mg":
                bra = slot.v(slot.ap[:, 0:512].rearrange("p (k n) -> p k n", k=4))
                brb = slot.v(slot.ap[:, 512:1024].rearrange("p (k n) -> p k n", k=4))
                ga = slot.v(slot.ap[:, 1024:2048].rearrange("p (k n) -> p k n", k=8))
                gb = slot.v(slot.ap[:, 2048:3072].rearrange("p (k n) -> p k n", k=8))
                S.dma("pool", bra, d_bra[l, :, i * 128:(i + 1) * 128].rearrange("(k p) n -> p k n", p=128))
                S.dma("pool", brb, d_brb[l, :, i * 128:(i + 1) * 128].rearrange("(k p) n -> p k n", p=128), join=True)
                S.dma("pool", ga, d_win[l, :, 3880 + i * 128:3880 + (i + 1) * 128].rearrange("(k p) n -> p k n", p=128), join=True)
                S.dma("pool", gb, d_win[l, :, 4904 + i * 128:4904 + (i + 1) * 128].rearrange("(k p) n -> p k n", p=128), join=True)
            elif kind == "wo":
                wv = k8(slot, 128)
                S.dma("pool", wv, d_wout[l, :, i * 128:(i + 1) * 128].rearrange("(k p) n -> p k n", p=128))

        def wtile(kind, l, i):
            slot = wslot()
            if cfg.prepass:
                S.dma("sp", slot[:, 0:WK[kind][1]], scr[kind][l][i])
            else:
                fill_f32(kind, slot, l, i)
            return slot

        def prepass():
            for l in range(DEPTH):
                for kind, (n_, c_) in WK.items():
                    for i in range(n_):
                        slot = wslot()
                        fill_f32(kind, slot, l, i)
                        S.dma("sp", scr[kind][l][i], slot[:, 0:c_])

        WAw = R([128, 512], BF16, "WAw")
        WAa = R([128, 512], BF16, "WAa")
        G0w = R([128, 512], BF16, "G0w")
        G1w = R([32, 512], BF16, "G1w")
        VUw = R([32, 512], BF16, "VUw")
        memset("dve", WAw, 0.0)
        memset("dve", WAa, 0.0)
        Wg = R([128, 8, 8], BF16, "Wg")

        HID_COLS = 22 * SEG
        ar_used = [0]
        ARENA_F32 = 23056
        arena = sb([128, ARENA_F32], F32, "arena")
        mix_regs = []

        def AR(parts, cols, dt=F32, name=""):
            ncol32 = cols if dt == F32 else (cols + 1) // 2
            a = arena[0:parts, ar_used[0]:ar_used[0] + ncol32]
            ar_used[0] += ncol32
            assert ar_used[0] <= ARENA_F32, "arena overflow"
            if dt != F32:
                a = a.bitcast(BF16)
            r = Reg(a, name)
            mix_regs.append(r)
            return r

        NF = 15
        Fw = [AR(128, 512, F32, f"F{i}") for i in range(3)]
        Bw = [AR(128, 512, BF16, f"B{i}") for i in range(2)]
        shared_regs = list(mix_regs)
        hid_off = ar_used[0]
        hid_bf = arena[:, hid_off:hid_off + HID_COLS // 2].bitcast(BF16)
        hid = [[Reg(hid_bf[:, (j * NTB + tb) * 512:(j * NTB + tb + 1) * 512]) for tb in range(NTB)] for j in range(22)]
        mix_regs = []
        Fw += [AR(128, 512, F32, f"F{i}") for i in range(3, NF)]
        LT = [AR(128, 512, F32, f"LT{i}") for i in range(2)]
        Bw += [AR(128, 512, BF16, f"B{i}") for i in range(2, 8)]
        ARpA = AR(128, 1024, BF16, "ARpA")
        ARpB = AR(128, 1024, BF16, "ARpB")
        vpad = AR(64, 2048, BF16, "vpad")
        Btp = AR(64, 2048, BF16, "Btp")
        Ktp = AR(64, 2048, BF16, "Ktp")
        Nb = [AR(64, 1024, BF16, f"Nb{h_}") for h_ in range(2)]
        Nk = [AR(64, 1024, BF16, f"Nk{h_}") for h_ in range(2)]
        Qb = [[AR(64, 512, BF16, f"Qb{q_}{i}") for i in range(2)] for q_ in range(2)]
        Pb = [[AR(64, 512, BF16, f"Pb{q_}{i}") for i in range(2)] for q_ in range(2)]
        Tq = [AR(64, 512, BF16, f"Tq{i}") for i in range(2)]
        Xs = AR(64, 128, BF16, "Xs")
        Unp = AR(64, 256, BF16, "Unp")
        Sbf = AR(128, 128, BF16, "Sbf")
        SPt = AR(128, 128, F32, "SPt")
        CNbf = AR(128, 256, BF16, "CNbf")
        CNe = AR(128, 256, F32, "CNe")
        zq = AR(128, 516, F32, "zq")
        zk = AR(128, 516, F32, "zk")
        vt1 = AR(64, 2048, BF16, "vt1")
        kto = AR(64, 1024, BF16, "kto")
        sTm = AR(64, 512, BF16, "sTm")
        Gg = [Fw[9 + i][0:4, :] for i in range(4)]
        hid_regs = [r for row in hid for r in row]

        def zero_pads():
            memset("dve", ARpA, 0.0)
            memset("dve", ARpB, 0.0)
            memset("dve", vpad, 0.0)
            memset("dve", Btp, 0.0)
            memset("dve", Ktp, 0.0)
            memset("dve", Unp, 0.0)
            memset("dve", vt1, 1.0)

        def to_mixer():
            S.transfer_hazards(hid_regs, mix_regs)
            zero_pads()

        def to_ffn():
            S.transfer_hazards(mix_regs, hid_regs)

        def zero_pads_unused():
            memset("dve", ARpA, 0.0)
            memset("dve", ARpB, 0.0)
            memset("dve", vpad, 0.0)
            memset("dve", Btp, 0.0)
            memset("dve", Ktp, 0.0)
            memset("dve", Unp, 0.0)
            memset("dve", vt1, 1.0)

        def rmsnorm(l, gbase, tb, dst=None):
            ps = P.alloc()
            for c in range(8):
                sq = Bw[c % 2] if dst is None else Bw[c % 2]
                act(sq, h[c][tb], AF.Square)
                mm(ps, ones_bf, sq, c == 0, c == 7)
            rstd = Fw[2]
            act(rstd, ps, AF.Sqrt, bias=1e-6, scale=1.0 / D)
            P.release(ps)
            recip(rstd, rstd)
            for c in range(8):
                o = xn[c][tb] if dst is None else h[c][tb]
                stt("dve", o, h[c][tb], pcol(l, gbase + c), rstd, MULT, MULT)

        def ffn(l, which, gbase):
            for tb in range(NTB):
                rmsnorm(l, gbase, tb)
            for j in range(22):
                wt = wtile("f%di" % which, l, j)
                wv = k8(wt, 256)
                for tb in range(NTB):
                    pg = P.alloc()
                    pu = P.alloc()
                    for k in range(8):
                        mm(pg, wv[:, k, 0:128], xn[k][tb], k == 0, k == 7)
                    for k in range(8):
                        mm(pu, wv[:, k, 128:256], xn[k][tb], k == 0, k == 7)
                    sg = Fw[j % 2]
                    act(sg, pg, AF.Silu)
                    tt("dve", hid[j][tb], sg, pu, MULT)
                    P.release(pg)
                    P.release(pu)
            for c in range(8):
                wt = wtile("f%do" % which, l, c)
                wv = wt.v(wt.ap[:, 0:22 * 128].rearrange("p (j n) -> p j n", j=22))
                for tb in range(NTB):
                    ps = P.alloc()
                    for j in range(22):
                        mm(ps, wv[:, j, :], hid[j][tb], j == 0, j == 21)
                    stt("dve", h[c][tb], ps, 0.5, h[c][tb], MULT, ADD)
                    P.release(ps)

        lt_rr = [0]

        def lerp(ps, rows, l, ci, out):
            tmp = LT[lt_rr[0] % 2]
            lt_rr[0] += 1
            mu = pv[0:rows, l, PV_MU + ci:PV_MU + ci + 1]
            omu = pvd[0:rows, l, ci:ci + 1]
            cr = car[l][ci]
            act(tmp[0:rows, 1:512], ps[0:rows, 0:511], AF.Copy, scale=mu)
            act(tmp[0:rows, 0:1], cr[0:rows, :], AF.Copy, scale=mu)
            stt("dve", out, ps[0:rows, :], omu, tmp[0:rows, :], MULT, ADD)
            act(cr[0:rows, :], ps[0:rows, 511:512], AF.Copy)

        def proj(wv, c0, c1, tb, rows=128):
            ps = P.alloc()
            for k in range(8):
                mm(ps[0:rows, :], wv[:, k, c0:c1], xn[k][tb], k == 0, k == 7)
            return ps

        def load_cols(wv, dst0, dsrc, l, col0, n, first):
            S.dma("pool", wv[:, :, dst0:dst0 + n], dsrc[l, :, col0:col0 + n].rearrange("(k p) n -> p k n", p=128),
                  join=not first)

        def mixer(l):
            for tb in range(NTB):
                rmsnorm(l, PV_MIX, tb)
            S.dma("pool", WAw[0:64, :], d_wup[l])
            S.dma("pool", WAa[64:128, :], d_aup[l])
            S.dma("pool", G0w, d_gup[l, 0:128, :])
            S.dma("pool", G1w, d_gup[l, 128:160, :])
            if l > 0:
                S.dma("pool", VUw, d_vu[l - 1])
            wt = wtile("sh", l, 0)
            wv = k8(wt, 320)
            S.dma("pool", Wg, d_win[l, :, 3872:3880].rearrange("(k p) n -> p k n", p=128))
            for tb in range(NTB):
                if cfg.do_rw:
                    ps = proj(wv, 0, 128, tb)
                    t_ = Fw[0]
                    lerp(ps, 128, l, 12, t_)
                    P.release(ps)
                    act(lora[0][tb][0:64, :], t_[0:64, :], AF.Tanh)
                    cp("act", lora[0][tb][64:128, :], t_[64:128, :])
                    ps = proj(wv, 128, 256, tb)
                    lerp(ps, 128, l, 13, t_)
                    P.release(ps)
                    act(lora[1][tb], t_, AF.Sigmoid)
                    ps = proj(wv, 256, 288, tb, rows=32)
                    lerp(ps, 32, l, 14, t_[0:32, :])
                    P.release(ps)
                    act(lora[2][tb][0:32, :], t_[0:32, :], AF.Sigmoid)
                    if l > 0:
                        ps = proj(wv, 288, 320, tb, rows=32)
                        cp("act", lora[3][tb][0:32, :], ps[0:32, :])
                        P.release(ps)
            if cfg.do_rw:
                for hp in range(4):
                    rwkv_pair(l, hp)
            else:
                for hp in range(4):
                    for tb in range(NTB):
                        memset("dve", ya[hp][tb], 0.0)
            if cfg.do_ml:
                for hd in range(4):
                    mlstm_head(l, hd)
            else:
                for hd in range(4):
                    for tb in range(NTB):
                        memset("dve", yb[hd][tb], 0.0)
            for oc in range(8):
                wt2 = wtile("mg", l, oc)
                bra = wt2.v(wt2.ap[:, 0:512].rearrange("p (k n) -> p k n", k=4))
                brb = wt2.v(wt2.ap[:, 512:1024].rearrange("p (k n) -> p k n", k=4))
                ga = wt2.v(wt2.ap[:, 1024:2048].rearrange("p (k n) -> p k n", k=8))
                gb = wt2.v(wt2.ap[:, 2048:3072].rearrange("p (k n) -> p k n", k=8))
                for tb in range(NTB):
                    pa = P.alloc()
                    for k in range(4):
                        mm(pa, bra[:, k, :], ya[k][tb], k == 0, k == 3)
                    pb = P.alloc()
                    for k in range(4):
                        mm(pb, brb[:, k, :], yb[k][tb], k == 0, k == 3)
                    pga = P.alloc()
                    for k in range(8):
                        mm(pga, ga[:, k, :], xn[k][tb], k == 0, k == 7)
                    t1, t2 = Fw[0], Fw[1]
                    act(t1, pga, AF.Sigmoid)
                    P.release(pga)
                    pgb = P.alloc()
                    for k in range(8):
                        mm(pgb, gb[:, k, :], xn[k][tb], k == 0, k == 7)
                    act(t2, pgb, AF.Sigmoid)
                    P.release(pgb)
                    tt("dve", t1, t1, pa, MULT)
                    tt("dve", t2, t2, pb, MULT)
                    tt("dve", u[oc][tb], t1, t2, ADD)
                    P.release(pa)
                    P.release(pb)
            for oc in range(8):
                wt2 = wtile("wo", l, oc)
                wo = k8(wt2, 128)
                for tb in range(NTB):
                    ps = P.alloc()
                    for k in range(8):
                        mm(ps, wo[:, k, :], u[k][tb], k == 0, k == 7)
                    tt("dve", h[oc][tb], h[oc][tb], ps, ADD)
                    P.release(ps)

        def rwkv_pair(l, hp):
            wt = wtile("rw", l, hp)
            wv = k8(wt, 384)
            Rr, Kk, Vv, SGW, Aa, KK, T1, T2, CUM, EP, EM, EX, BON, Gt, Yt = Fw
            Rt, Kt, Bt, At, Vbf, Tb1, Tb2, _ = Bw
            ARp = (ARpA, ARpB)
            csl = slice(hp * 128, (hp + 1) * 128)
            for tb in range(NTB):
                for idx, (dst, ci) in enumerate(((Rr, hp), (Kk, 4 + hp), (Vv, 8 + hp))):
                    ps = proj(wv, idx * 128, (idx + 1) * 128, tb)
                    lerp(ps, 128, l, ci, dst)
                    P.release(ps)
                ps = P.alloc()
                mm(ps, WAw[:, csl], lora[0][tb])
                act(SGW, ps, AF.Sigmoid, bias=pcol(l, PV_W0 + hp))
                P.release(ps)
                ps = P.alloc()
                mm(ps, WAa[:, csl], lora[0][tb])
                act(Aa, ps, AF.Sigmoid, bias=pcol(l, PV_A0 + hp))
                P.release(ps)
                ps = P.alloc()
                mm(ps, G0w[:, csl], lora[1][tb], True, False)
                mm(ps, G1w[0:32, csl], lora[2][tb][0:32, :], False, True)
                cp("act", Gt, ps)
                P.release(ps)
                if l > 0:
                    ps = P.alloc()
                    mm(ps, VUw[0:32, csl], lora[3][tb][0:32, :])
                    act(T1, ps, AF.Sigmoid, bias=pcol(l, PV_VB + hp))
                    P.release(ps)
                    tt("dve", T2, vfirst[hp][tb], Vv, SUB)
                    tt("dve", T2, T2, T1, MULT)
                    tt("dve", Vv, Vv, T2, ADD)
                else:
                    cp("act", vfirst[hp][tb], Vv)
                ts("dve", KK, Kk, pcol(l, PV_KK + hp), None, MULT)
                tt("dve", Tb1, KK, KK, MULT)
                ps = P.alloc()
                mm(ps, bd_bf, Tb1)
                act(T1, ps, AF.Sqrt)
                P.release(ps)
                ts("dve", T1, T1, 1e-12, None, MAXOP)
                recip(T1, T1)
                tt("dve", KK, KK, T1, MULT)
                ts("dve", T1, Aa, pcol(l, PV_KA + hp), pvd[:, l, 15 + hp:16 + hp], MULT, ADD)
                tt("dve", Kk, Kk, T1, MULT)
                stt("dve", Tb2, Rr, pcol(l, PV_RK + hp), Kk, MULT, MULT)
                ps = P.alloc()
                mm(ps, bd_bf, Tb2)
                tt("dve", BON, ps, Vv, MULT)
                P.release(ps)
                scan(CUM, resetm, SGW)
                act(EP, CUM, AF.Exp, scale=-C0)
                act(EM, CUM, AF.Exp, scale=C0)
                tt("dve", T1, CUM, SGW, SUB)
                act(EX, T1, AF.Exp, scale=-C0)
                tt("dve", Rt, Rr, EP, MULT)
                tt("dve", Kt, Kk, EM, MULT)
                tt("dve", T1, KK, Aa, MULT)
                tt("dve", Bt, T1, EM, MULT)
                tt("dve", At, KK, EX, MULT)
                cp("act", Vbf, Vv)
                for h_ in range(2):
                    psl = slice(64 * h_, 64 * h_ + 64)
                    av = ARp[h_].v(ARp[h_].ap.rearrange("p (c two t) -> p c two t", c=8, two=2))
                    cp("act", av[psl, :, 0, :], At.v(At.ap.rearrange("p (c t) -> p c t", c=8))[psl])
                    cp("act", av[psl, :, 1, :], Rt.v(Rt.ap.rearrange("p (c t) -> p c t", c=8))[psl])
                for src, dstp in ((Vbf, vpad), (Bt, Btp), (Kt, Ktp)):
                    dv = dstp.v(dstp.ap.rearrange("p (c two n) -> p c two n", c=8, two=2))
                    for q in range(2):
                        ps = P.alloc()
                        for cc in range(4):
                            c = q * 4 + cc
                            mm(ps[0:64, cc * 128:(cc + 1) * 128], src[:, c * 64:(c + 1) * 64], ident_bf)
                        psv = ps.v(ps.ap[0:64, :].rearrange("p (c n) -> p c n", c=4))
                        cp("act", dv[:, q * 4:q * 4 + 4, 0, 0:64], psv[:, :, 0:64])
                        cp("dve", dv[:, q * 4:q * 4 + 4, 1, 64:128], psv[:, :, 64:128])
                        P.release(ps)
                NbV = [x.v(x.ap.rearrange("p (c n) -> p c n", c=8)) for x in Nb]
                NkV = [x.v(x.ap.rearrange("p (c n) -> p c n", c=8)) for x in Nk]
                ARV = [x.v(x.ap.rearrange("p (c n) -> p c n", c=8)) for x in ARp]
                idv = identrep.rearrange("p (c t) -> p c t", c=8)
                def blk(x, i8):
                    return x[:, i8 * 64:(i8 + 1) * 64]

                for q in range(2):
                    for h_ in range(2):
                        psb = P.alloc()
                        psk = P.alloc()
                        for cc in range(4):
                            c = q * 4 + cc
                            mm(psb[0:64, cc * 128:(cc + 1) * 128], Bt[:, c * 64:(c + 1) * 64], ARV[h_][:, c, :])
                            mm(psk[0:64, cc * 128:(cc + 1) * 128], Kt[:, c * 64:(c + 1) * 64], ARV[h_][:, c, :])
                        tt("dve", Nb[h_][:, q * 512:(q + 1) * 512], psb[0:64, :], mask_si, MULT)
                        tt("dve", Nk[h_][:, q * 512:(q + 1) * 512], psk[0:64, :], mask_si, MULT)
                        P.release(psb)
                        P.release(psk)
                    pst = P.alloc()
                    for h_ in range(2):
                        for cc in range(4):
                            c = q * 4 + cc
                            i8 = h_ * 4 + cc
                            mm(pst[0:64, i8 * 64:(i8 + 1) * 64], ARV[h_][:, c, 0:64], Bt[:, c * 64:(c + 1) * 64])
                    tt("dve", Qb[q][0], pst[0:64, :], mask_sl, MULT)
                    P.release(pst)
                    TtV = Tq[q].v(Tq[q].ap.rearrange("p (h c t) -> p h c t", h=2, c=4))
                    for h_ in range(2):
                        tt("dve", TtV[:, h_, :, :], idv[:, 0:4, :], NbV[h_][:, q * 4:q * 4 + 4, 0:64], SUB)
                Pm = [(lambda q_: (lambda i8: NbV[i8 // 4][:, q_ * 4 + i8 % 4, 0:64]))(q) for q in range(2)]
                Qm = [(lambda q_: (lambda i8: blk(Qb[q_][0], i8)))(q) for q in range(2)]
                for lev in range(5):
                    for q in range(2):
                        Tt = Tq[q]
                        newQ = Qb[q][(lev + 1) % 2]
                        psq = P.alloc()
                        for i8 in range(8):
                            mm(blk(psq[0:64, :], i8), Pm[q](i8), Qm[q](i8))
                        cp("act", newQ, psq[0:64, :])
                        P.release(psq)
                        if lev < 4:
                            newP = Pb[q][lev % 2]
                            psp = P.alloc()
                            for i8 in range(8):
                                mm(blk(psp[0:64, :], i8), Qm[q](i8), Pm[q](i8))
                            cp("act", newP, psp[0:64, :])
                            P.release(psp)
                        pt2 = P.alloc()
                        for i8 in range(8):
                            mm(blk(pt2[0:64, :], i8), blk(newQ, i8), blk(Tt, i8))
                        tt("dve", Tt, Tt, pt2[0:64, :], ADD)
                        P.release(pt2)
                        if lev < 4:
                            Pm[q] = (lambda np_: (lambda i8: blk(np_, i8)))(newP)
                        Qm[q] = (lambda nq_: (lambda i8: blk(nq_, i8)))(newQ)
                Sreg = Sst[l][hp]
                cp("act", Sbf, Sreg)
                vpv = vpad.v(vpad.ap.rearrange("p (c two n) -> p c two n", c=8, two=2))
                bpv = Btp.v(Btp.ap.rearrange("p (c two n) -> p c two n", c=8, two=2))
                kpv = Ktp.v(Ktp.ap.rearrange("p (c two n) -> p c two n", c=8, two=2))
                unv = Unp.v(Unp.ap.rearrange("p (two n) -> p two n", two=2))
                un4 = Unp.v(Unp.ap.rearrange("p (b n) -> p b n", b=4))
                psY = P.alloc()
                for c in range(8):
                    q, cc = c // 4, c % 4
                    ck = slice(c * 64, (c + 1) * 64)
                    psX = P.alloc()
                    mm(psX[0:64, 0:128], At[:, ck], Sbf, True, False)
                    mm(psX[0:64, 0:128], NkV[0][:, c, 0:64], vpv[:, c, 0, :], False, False)
                    mm(psX[0:64, 0:128], NkV[1][:, c, 0:64], vpv[:, c, 1, :], False, True)
                    cp("act", Xs, psX[0:64, 0:128])
                    P.release(psX)
                    psU = P.alloc()
                    mm(psU[0:64, 0:64], blk(Tq[q], cc), Xs[:, 0:64])
                    mm(psU[0:64, 64:128], blk(Tq[q], 4 + cc), Xs[:, 64:128])
                    ts("dve", un4[:, 0:4:3, :], psU.v(psU.ap[0:64, 0:128].rearrange("p (two n) -> p two n", two=2)),
                       -1.0, None, MULT)
                    P.release(psU)
                    mm(psY[:, ck], Sbf, Rt[:, ck], True, False)
                    mm(psY[:, ck], unv[:, 0, :], NbV[0][:, c, 64:128], False, False)
                    mm(psY[:, ck], unv[:, 1, :], NbV[1][:, c, 64:128], False, False)
                    mm(psY[:, ck], vpv[:, c, 0, :], NkV[0][:, c, 64:128], False, False)
                    mm(psY[:, ck], vpv[:, c, 1, :], NkV[1][:, c, 64:128], False, True)
                    psS = P.alloc()
                    mm(psS[:, 0:128], bpv[:, c, 0, :], unv[:, 0, :], True, False)
                    mm(psS[:, 0:128], bpv[:, c, 1, :], unv[:, 1, :], False, False)
                    mm(psS[:, 0:128], kpv[:, c, 0, :], vpv[:, c, 0, :], False, False)
                    mm(psS[:, 0:128], kpv[:, c, 1, :], vpv[:, c, 1, :], False, True)
                    pL = EP[:, c * 64 + 63:c * 64 + 64]
                    ts("dve", SPt, Sreg, pL, None, MULT)
                    stt("dve", Sbf, psS[:, 0:128], pL, SPt, MULT, ADD)
                    stt("dve", Sreg, psS[:, 0:128], pL, SPt, MULT, ADD)
                    P.release(psS)
                cp("act", Yt, psY)
                P.release(psY)
                cp("act", Tb1, Yt)
                tt("dve", Tb2, Yt, Yt, MULT)
                ps1 = P.alloc()
                mm(ps1, bd_bf, Tb1)
                ps2 = P.alloc()
                mm(ps2, bd_bf, Tb2)
                ts("dve", T1, ps1, 1.0 / 64, None, MULT)
                tt("dve", T2, T1, T1, MULT)
                stt("dve", T2, ps2, 1.0 / 64, T2, MULT, SUB)
                P.release(ps1)
                P.release(ps2)
                ts("dve", T2, T2, 0.0, None, MAXOP)
                act(T2, T2, AF.Sqrt, bias=64e-5, scale=1.0)
                recip(T2, T2)
                tt("dve", Yt, Yt, T1, SUB)
                tt("dve", Yt, Yt, T2, MULT)
                ts("dve", Yt, Yt, pcol(l, PV_GNW + hp), pcol(l, PV_GNB + hp), MULT, ADD)
                tt("dve", Yt, Yt, BON, ADD)
                tt("dve", ya[hp][tb], Yt, Gt, MULT)

        def mlstm_head(l, hd):
            LI, LF, BC, G1 = Gg
            GT = LF
            wt = wtile("ml", l, hd)
            wv = k8(wt, 512)
            Qc, Kc, SO, ALPHA, BETA, HH, T1, T2, MEAN = Fw[0:9]
            QT, KT, Vbf, Tb1, Tb2 = Bw[0:5]
            CN = CNst[l][hd]
            for tb in range(NTB):
                if hd == 0 or NTB > 1:
                    ps = proj(Wg, 0, 4, tb, rows=4)
                    ts("dve", LI, ps[0:4, :], pv[0:4, l, PV_IB:PV_IB + 1], None, ADD)
                    P.release(ps)
                    ps = proj(Wg, 4, 8, tb, rows=4)
                    act(GT, ps[0:4, :], AF.Exp, bias=pvd[0:4, l, 19:20], scale=-1.0)
                    P.release(ps)
                    act(GT, GT, AF.Ln, bias=1.0)
                    ts("dve", LF, GT, -1.0, None, MULT)
                    scan(BC, resetm[0:4, :], LF)
                    tt("dve", G1, LI, BC, SUB)
                for (zz, dst, idx) in ((zq, Qc, 0), (zk, Kc, 1)):
                    ci = idx * 4 + hd
                    ps = proj(wv, idx * 128, (idx + 1) * 128, tb)
                    cp("act", zz[:, 3:515], ps)
                    P.release(ps)
                    cp("act", zz[:, 0:3], cqk[l][ci])
                    ts("dve", dst, zz[:, 0:512], pcol(l, PV_CW + ci), pcol(l, PV_CB + ci), MULT, ADD)
                    for j in range(1, 4):
                        stt("dve", dst, zz[:, j:j + 512], pcol(l, PV_CW + 8 * j + ci), dst, MULT, ADD)
                    cp("act", cqk[l][ci], zz[:, 512:515])
                    act(dst, dst, AF.Silu)
                ps = proj(wv, 256, 384, tb)
                cp("act", Vbf, ps)
                P.release(ps)
                ps = proj(wv, 384, 512, tb)
                act(SO, ps, AF.Sigmoid)
                P.release(ps)
                ps = P.alloc()
                mm(ps, sel4[:, hd * 128:(hd + 1) * 128], BC)
                act(ALPHA, ps, AF.Exp)
                P.release(ps)
                ps = P.alloc()
                mm(ps, sel4[:, hd * 128:(hd + 1) * 128], G1)
                act(BETA, ps, AF.Exp)
                P.release(ps)
                stt("dve", QT, Qc, 128.0 ** -0.5, ALPHA, MULT, MULT)
                tt("dve", KT, Kc, BETA, MULT)
                v1 = vt1.v(vt1.ap.rearrange("p (c n) -> p c n", c=8))
                ktv = kto.v(kto.ap.rearrange("p (c n) -> p c n", c=8))
                for q in range(2):
                    ps = P.alloc()
                    for cc in range(4):
                        c = q * 4 + cc
                        mm(ps[0:64, cc * 128:(cc + 1) * 128], Vbf[:, c * 64:(c + 1) * 64], ident_bf)
                    cp("act", v1[:, q * 4:q * 4 + 4, 0:128], ps.v(ps.ap[0:64, :].rearrange("p (c n) -> p c n", c=4)))
                    P.release(ps)
                    ps = P.alloc()
                    for cc in range(4):
                        c = q * 4 + cc
                        mm(ps[0:64, cc * 128:(cc + 1) * 128], KT[:, c * 64:(c + 1) * 64], ident_bf)
                    cp("act", kto[:, q * 512:(q + 1) * 512], ps[0:64, :])
                    P.release(ps)
                ps = P.alloc()
                for c in range(8):
                    ck = slice(c * 64, (c + 1) * 64)
                    mm(ps[0:64, ck], KT[:, ck], QT[:, ck])
                tt("dve", sTm, ps[0:64, :], mask_incl, MULT)
                P.release(ps)
                cp("act", CNbf, CN)
                psN = P.alloc()
                psD = P.alloc()
                for c in range(8):
                    ck = slice(c * 64, (c + 1) * 64)
                    mm(psN[:, ck], v1[:, c, 0:128], sTm[:, ck], True, False)
                    mm(psN[:, ck], CNbf[:, 0:128], QT[:, ck], False, True)
                    mm(psD[:, ck], ones_bf[0:64, :], sTm[:, ck], True, False)
                    mm(psD[:, ck], CNbf[:, 128:256], QT[:, ck], False, True)
                    psC = P.alloc()
                    mm(psC[:, 0:256], ktv[:, c, :], v1[:, c, :])
                    ebL = ALPHA[:, c * 64 + 63:c * 64 + 64]
                    ts("dve", CNe, CN, ebL, None, MULT)
                    stt("dve", CNbf, psC[:, 0:256], ebL, CNe, MULT, ADD)
                    stt("dve", CN, psC[:, 0:256], ebL, CNe, MULT, ADD)
                    P.release(psC)
                act(T1, psD, AF.Abs)
                P.release(psD)
                ts("dve", T1, T1, 1.0, None, MAXOP)
                recip(T1, T1)
                tt("dve", HH, psN, T1, MULT)
                P.release(psN)
                cp("act", Tb1, HH)
                tt("dve", Tb2, HH, HH, MULT)
                ps1 = P.alloc()
                mm(ps1, ones_bf, Tb1)
                ps2 = P.alloc()
                mm(ps2, ones_bf, Tb2)
                ts("dve", MEAN, ps1, 1.0 / 128, None, MULT)
                tt("dve", T2, MEAN, MEAN, MULT)
                stt("dve", T2, ps2, 1.0 / 128, T2, MULT, SUB)
                P.release(ps1)
                P.release(ps2)
                ts("dve", T2, T2, 0.0, None, MAXOP)
                act(T2, T2, AF.Sqrt, bias=1e-5, scale=1.0)
                recip(T2, T2)
                tt("dve", HH, HH, MEAN, SUB)
                tt("dve", HH, HH, T2, MULT)
                stt("dve", yb[hd][tb], HH, pcol(l, PV_NW + hd), SO, MULT, MULT)

        out_toks = []
        if cfg.prepass:
            prepass()
        for s in range(NS):
            for rg in state_all:
                memset("dve", rg, 0.0)
            for g in range(NSEG):
                t0 = g * SEG
                for c in range(8):
                    for tb in range(NTB):
                        S.dma("sp", h[c][tb], xT[s, c * 128:(c + 1) * 128, t0 + tb * 512:t0 + (tb + 1) * 512])
                for l in range(DEPTH):
                    if cfg.do_ffn:
                        to_ffn()
                        ffn(l, 1, PV_FFN1)
                    if cfg.do_mix:
                        to_mixer()
                        mixer(l)
                    if cfg.do_ffn:
                        to_ffn()
                        ffn(l, 2, PV_FFN2)
                for tb in range(NTB):
                    rmsnorm(0, PV_FIN, tb, dst="h")
                    for c in range(8):
                        out_toks.append(S.dma("sp", outT[s, c * 128:(c + 1) * 128, t0 + tb * 512:t0 + (tb + 1) * 512],
                                              h[c][tb]))
        S.wait_all_at_end("sp", [("dma", k, 16 * S.dma_cnt[k]) for k in range(S.n_dma_sems) if S.dma_cnt[k]])
        S.emit(st)
    return nc


_CACHE = {}


def run_cores(inp, cfg, n_cores):
    depth = cfg.DEPTH
    key = (cfg.NS, cfg.T, cfg.DEPTH, cfg.NTB, cfg.do_ffn, cfg.do_rw, cfg.do_ml, cfg.do_mix, cfg.prepass)
    if key not in _CACHE:
        _CACHE[key] = build_program(cfg)
    nc = _CACHE[key]
    pv = pack_params(inp, depth)
    consts = make_consts()
    def f(k):
        a = np.ascontiguousarray(inp[k], dtype=np.float32)
        if a.shape[0] == 0:
            a = np.zeros((1,) + a.shape[1:], np.float32)
        return a
    shared = {
        "ffn1_w_in": f("ffn1_w_in"), "ffn1_w_out": f("ffn1_w_out"), "ffn2_w_in": f("ffn2_w_in"),
        "ffn2_w_out": f("ffn2_w_out"), "w_in": f("w_in"), "rw_w_up": f("rw_w_up"), "rw_a_up": f("rw_a_up"),
        "rw_g_up": f("rw_g_up"), "vres_down": f("vres_down"), "vres_up": f("vres_up"), "br_a": f("br_a"),
        "br_b": f("br_b"), "w_out": f("w_out"), "pvec": pv, "consts": consts,
    }
    x = np.asarray(inp["x"], np.float32)
    in_maps = []
    for c in range(n_cores):
        xs = x[c * cfg.NS:(c + 1) * cfg.NS]
        m = dict(shared)
        m["xT"] = np.ascontiguousarray(xs.transpose(0, 2, 1))
        in_maps.append(m)
    res = run_bass_kernel_spmd(nc, in_maps, core_ids=list(range(n_cores)))
    outs = [np.asarray(r["outT"]).transpose(0, 2, 1) for r in res.results]
    return np.ascontiguousarray(np.concatenate(outs, axis=0), dtype=np.float32)


def kernel(**inputs):
    cfg = Cfg(NS=4, T=2048, DEPTH=4, NTB=1)
    return run_cores(inputs, cfg, 8)
```
